# Optimizing a Trainium2 kernel written in Bass

```python
import jax, jax.numpy as jnp
from jax import lax
import numpy as np

D_MODEL = 2048
BATCH = 1
SEQ = 16384
DEPTH = 2

N_MIXERS = 2
MEM_TOKENS = 256
MEM_WIDTH = D_MODEL // 4
MEM_HEADS = 4
MEM_HEAD_DIM = MEM_WIDTH // MEM_HEADS
MIXER_WIDTH = D_MODEL - MEM_WIDTH
HEAD_DIM = 128
ATT_HEADS = MIXER_WIDTH // HEAD_DIM
ROPE_THETA = 500000.0
ROT_FRACTION = 4
IDX_HEADS = 16
IDX_DIM = 64
TOPK_MAX = 256
Q_BLOCK = 128
HG_HEADS = 12
HG_DK = MIXER_WIDTH // HG_HEADS
HG_DV = MIXER_WIDTH // HG_HEADS
HG_CHUNK = 64
D_FF = ((8 * D_MODEL // 3 + 255) // 256) * 256
CONV_WIDTH = 3
DSA_IN = 3 * MIXER_WIDTH + IDX_HEADS * IDX_DIM + IDX_DIM + IDX_HEADS + MEM_WIDTH
HG_IN = 4 * MIXER_WIDTH + MEM_WIDTH
N_DSA_LAYERS = (DEPTH + N_MIXERS - 1) // N_MIXERS
N_HG_LAYERS = DEPTH // N_MIXERS
NORM_EPS = 1e-6
NEG_INF = -1e30

kernel_name = 'hybrid_dsa_hgrn2_memory_convffn'


def rms_norm(x, w):
    xf = x.astype(jnp.float32)
    y = xf * lax.rsqrt(jnp.mean(xf * xf, axis=-1, keepdims=True) + NORM_EPS)
    return (y * w.astype(jnp.float32)).astype(x.dtype)


def split_cols(a, sizes):
    out, start = [], 0
    for s in sizes:
        out.append(a[..., start:start + s])
        start += s
    return out


def partial_rope(x, positions):
    d = x.shape[-1]
    rot = d // ROT_FRACTION
    half = rot // 2
    inv_freq = ROPE_THETA ** (-jnp.arange(half, dtype=jnp.float32) / half)
    ang = positions.astype(jnp.float32)[..., None] * inv_freq
    ang = ang.reshape(ang.shape[:2] + (1,) * (x.ndim - 3) + (half,))
    cos, sin = jnp.cos(ang), jnp.sin(ang)
    x1 = x[..., :half].astype(jnp.float32)
    x2 = x[..., half:rot].astype(jnp.float32)
    rotated = jnp.concatenate([x1 * cos - x2 * sin, x2 * cos + x1 * sin], axis=-1).astype(x.dtype)
    return jnp.concatenate([rotated, x[..., rot:]], axis=-1)


def dsa_sparse_attention(proj, positions):
    B, T, _ = proj.shape
    n_sel = min(TOPK_MAX, T // 4)
    nb = T // Q_BLOCK
    q, k, v, iq, ik, iw = split_cols(
        proj, [MIXER_WIDTH, MIXER_WIDTH, MIXER_WIDTH, IDX_HEADS * IDX_DIM, IDX_DIM, IDX_HEADS])
    q = partial_rope(q.reshape(B, T, ATT_HEADS, HEAD_DIM), positions)
    k = partial_rope(k.reshape(B, T, ATT_HEADS, HEAD_DIM), positions)
    v = v.reshape(B, T, ATT_HEADS, HEAD_DIM)
    iq = partial_rope(iq.reshape(B, T, IDX_HEADS, IDX_DIM), positions)
    ik = partial_rope(ik, positions)
    iw = iw * (IDX_HEADS ** -0.5 * IDX_DIM ** -0.5)
    key_pos = jnp.arange(T, dtype=jnp.int32)
    att_scale = HEAD_DIM ** -0.5

    def to_blocks(a):
        return jnp.moveaxis(a.reshape((B, nb, Q_BLOCK) + a.shape[2:]), 1, 0)

    def block(args):
        q_b, iq_b, iw_b, start = args
        q_pos = start + jnp.arange(Q_BLOCK, dtype=jnp.int32)
        s = jnp.einsum('bqhd,bsd->bqhs', iq_b, ik, preferred_element_type=jnp.float32)
        score = jnp.einsum('bqhs,bqh->bqs', jax.nn.relu(s), iw_b.astype(jnp.float32))
        causal = key_pos[None, :] <= q_pos[:, None]
        score = jnp.where(causal[None], score, NEG_INF)
        _, sel = lax.top_k(score, n_sel)
        valid = sel <= q_pos[None, :, None]
        k_g = jax.vmap(lambda kb, ib: kb[ib])(k, sel)
        v_g = jax.vmap(lambda vb, ib: vb[ib])(v, sel)
        logits = jnp.einsum('bqhd,bqkhd->bqhk', q_b, k_g, preferred_element_type=jnp.float32) * att_scale
        logits = jnp.where(valid[:, :, None, :], logits, NEG_INF)
        p = jax.nn.softmax(logits, axis=-1).astype(v.dtype)
        return jnp.einsum('bqhk,bqkhd->bqhd', p, v_g)

    starts = jnp.arange(nb, dtype=jnp.int32) * Q_BLOCK
    out = lax.map(block, (to_blocks(q), to_blocks(iq), to_blocks(iw), starts))
    return jnp.moveaxis(out, 0, 1).reshape(B, T, MIXER_WIDTH)


def hgrn_lower_bound(lb_logits, layer):
    cs = jnp.cumsum(jax.nn.softmax(lb_logits.astype(jnp.float32), axis=0), axis=0)
    return cs[layer] - cs[0]


def hgrn2_recurrence(proj, lower_bound, gnorm_w):
    B, T, _ = proj.shape
    q, f, i, og = split_cols(proj, [MIXER_WIDTH] * 4)
    q = jax.nn.silu(q.astype(jnp.float32))
    forget = lower_bound + (1.0 - lower_bound) * jax.nn.sigmoid(f.astype(jnp.float32))
    kin = 1.0 - forget
    g = jnp.log(forget)
    n = T // HG_CHUNK

    def to_chunks(a, d):
        return a.reshape(B, n, HG_CHUNK, HG_HEADS, d).transpose(1, 0, 3, 2, 4)

    qs, ks, gs = to_chunks(q, HG_DK), to_chunks(kin, HG_DK), to_chunks(g, HG_DK)
    vs = to_chunks(i.astype(jnp.float32), HG_DV)
    tri = jnp.tril(jnp.ones((HG_CHUNK, HG_CHUNK), dtype=bool))

    def step(S, xs):
        qc, kc, vc, gc = xs
        b = jnp.cumsum(gc, axis=2)
        o_inter = jnp.einsum('bhtk,bhkv->bhtv', qc * jnp.exp(b), S)
        diff = b[:, :, :, None, :] - b[:, :, None, :, :]
        decay = jnp.exp(jnp.where(tri[:, :, None], diff, -jnp.inf))
        a = jnp.einsum('bhtk,bhsk,bhtsk->bhts', qc, kc, decay)
        o_intra = jnp.einsum('bhts,bhsv->bhtv', a, vc)
        b_last = b[:, :, -1:, :]
        S = jnp.exp(b_last[:, :, 0, :, None]) * S + jnp.einsum(
            'bhsk,bhsv->bhkv', kc * jnp.exp(b_last - b), vc)
        return S, o_inter + o_intra

    S0 = jnp.zeros((B, HG_HEADS, HG_DK, HG_DV), jnp.float32)
    _, o = lax.scan(step, S0, (qs, ks, vs, gs))
    o = o.transpose(1, 0, 3, 2, 4).reshape(B, T, HG_HEADS, HG_DV)
    o = o * lax.rsqrt(jnp.mean(o * o, axis=-1, keepdims=True) + NORM_EPS) * gnorm_w.astype(jnp.float32)
    gate = jax.nn.silu(og.astype(jnp.float32)).reshape(B, T, HG_HEADS, HG_DV)
    return (o * gate).reshape(B, T, MIXER_WIDTH).astype(proj.dtype)


def memory_cross_attention(mq, mem_k, mem_v):
    B, T, _ = mq.shape
    q = mq.reshape(B, T, MEM_HEADS, MEM_HEAD_DIM)
    logits = jnp.einsum('bthd,bmhd->bhtm', q, mem_k, preferred_element_type=jnp.float32) * MEM_HEAD_DIM ** -0.5
    p = jax.nn.softmax(logits, axis=-1).astype(mem_v.dtype)
    return jnp.einsum('bhtm,bmhd->bthd', p, mem_v).reshape(B, T, MEM_WIDTH)


def causal_depthwise_conv(h, w, b):
    F = h.shape[-1]
    y = lax.conv_general_dilated(
        h, w[:, None, :].astype(h.dtype), window_strides=(1,), padding=[(CONV_WIDTH - 1, 0)],
        dimension_numbers=('NWC', 'WIO', 'NWC'), feature_group_count=F)
    return y + b.astype(h.dtype)


def conv_ffn(h, w_gate, w_up, conv_w, conv_b, w_down):
    gt = causal_depthwise_conv(h @ w_gate, conv_w, conv_b)
    return (jax.nn.silu(gt) * (h @ w_up)) @ w_down


def setup_inputs(seed: int = 0) -> dict:
    key = jax.random.key(seed)
    ks = jax.random.split(key, 20)
    f32 = jnp.float32

    def nrm(k, shape, fan_in):
        return jax.random.normal(k, shape, f32) * fan_in ** -0.5

    def gain(k, shape):
        return 1.0 + 0.02 * jax.random.normal(k, shape, f32)

    offset = jax.random.randint(ks[2], (BATCH, 1), 0, 4096, dtype=jnp.int32)
    positions = offset + jnp.arange(SEQ, dtype=jnp.int32)[None, :]
    return {
        'x': jax.random.normal(ks[0], (BATCH, SEQ, D_MODEL), f32),
        'mem': jax.random.normal(ks[1], (BATCH, MEM_TOKENS, D_MODEL), f32),
        'positions': positions,
        'ln1_w': gain(ks[3], (DEPTH, D_MODEL)),
        'ln2_w': gain(ks[4], (DEPTH, D_MODEL)),
        'dsa_w_in': nrm(ks[5], (N_DSA_LAYERS, D_MODEL, DSA_IN), D_MODEL),
        'hgrn_w_in': nrm(ks[6], (N_HG_LAYERS, D_MODEL, HG_IN), D_MODEL),
        'hgrn_lb_logits': 0.1 * jax.random.normal(ks[7], (DEPTH, HG_HEADS * HG_DK), f32),
        'hgrn_gnorm_w': gain(ks[8], (N_HG_LAYERS, HG_DV)),
        'mem_norm_w': gain(ks[9], (D_MODEL,)),
        'w_mem_kv': nrm(ks[10], (D_MODEL, 2 * MEM_WIDTH), D_MODEL),
        'w_out': nrm(ks[11], (DEPTH, D_MODEL, D_MODEL), D_MODEL),
        'ffn_w_gate': nrm(ks[12], (DEPTH, D_MODEL, D_FF), D_MODEL),
        'ffn_w_up': nrm(ks[13], (DEPTH, D_MODEL, D_FF), D_MODEL),
        'ffn_conv_w': nrm(ks[14], (DEPTH, CONV_WIDTH, D_FF), CONV_WIDTH),
        'ffn_conv_b': 0.01 * jax.random.normal(ks[15], (DEPTH, D_FF), f32),
        'ffn_w_down': nrm(ks[16], (DEPTH, D_FF, D_MODEL), D_FF),
        'final_norm_w': gain(ks[17], (D_MODEL,)),
    }


def reference(x, mem, positions, ln1_w, ln2_w, dsa_w_in, hgrn_w_in, hgrn_lb_logits, hgrn_gnorm_w,
              mem_norm_w, w_mem_kv, w_out, ffn_w_gate, ffn_w_up, ffn_conv_w, ffn_conv_b, ffn_w_down,
              final_norm_w):
    B, M, _ = mem.shape
    mem_kv = rms_norm(mem, mem_norm_w) @ w_mem_kv
    mem_k = mem_kv[..., :MEM_WIDTH].reshape(B, M, MEM_HEADS, MEM_HEAD_DIM)
    mem_v = mem_kv[..., MEM_WIDTH:].reshape(B, M, MEM_HEADS, MEM_HEAD_DIM)
    for layer in range(DEPTH):
        h = rms_norm(x, ln1_w[layer])
        j = layer // N_MIXERS
        if layer % N_MIXERS == 0:
            proj = h @ dsa_w_in[j]
            mix = dsa_sparse_attention(proj, positions)
        else:
            proj = h @ hgrn_w_in[j]
            mix = hgrn2_recurrence(proj, hgrn_lower_bound(hgrn_lb_logits, layer), hgrn_gnorm_w[j])
        mem_out = memory_cross_attention(proj[..., -MEM_WIDTH:], mem_k, mem_v)
        x = x + jnp.concatenate([mix, mem_out], axis=-1) @ w_out[layer]
        h = rms_norm(x, ln2_w[layer])
        x = x + conv_ffn(h, ffn_w_gate[layer], ffn_w_up[layer], ffn_conv_w[layer], ffn_conv_b[layer],
                         ffn_w_down[layer])
    return rms_norm(x, final_norm_w)
```

```python
import math
import numpy as np
import ml_dtypes
import concourse.bass as bass
import concourse.mybir as mybir
from concourse.bass_utils import run_bass_kernel_spmd

F32 = mybir.dt.float32
BF16 = mybir.dt.bfloat16
I32 = mybir.dt.int32
ALU = mybir.AluOpType
AF = mybir.ActivationFunctionType
AX = mybir.AxisListType
NPBF = ml_dtypes.bfloat16

NCORES = 8
D = 2048
SEQ = 16384
T = SEQ // NCORES
NT = T // 128
MW = 1536
DSA_IN = 6224
HG_IN = 6656
DFF = 5632
NFB = DFF // 128
EPS = 1e-6
TWO_PI = 2.0 * math.pi


class FW:
    def __init__(self, nc):
        self.nc = nc
        self.E = {"pe": nc.tensor, "dve": nc.vector, "act": nc.scalar, "pool": nc.gpsimd, "sp": nc.sync}
        self.sem = {k: nc.alloc_semaphore("s_" + k) for k in ("pe", "dve", "act", "pool")}
        self.cnt = {k: 0 for k in self.sem}
        self.seen = {e: {} for e in self.E}
        self.res = {}
        self.lanes = {}
        self.n_sb = 0
        self.trace = {e: [] for e in self.E}

    def check_deadlock(self):
        sems = {}
        pc = {e: 0 for e in self.E}
        progress = True
        while progress:
            progress = False
            for e, tr in self.trace.items():
                while pc[e] < len(tr):
                    kind, key, val = tr[pc[e]]
                    if kind == "wait":
                        if sems.get(key, 0) >= val:
                            pc[e] += 1
                            progress = True
                        else:
                            break
                    else:
                        sems[key] = sems.get(key, 0) + val
                        pc[e] += 1
                        progress = True
        stuck = {e: (pc[e], len(tr), tr[pc[e]] if pc[e] < len(tr) else None) for e, tr in self.trace.items()}
        return all(pc[e] == len(tr) for e, tr in self.trace.items()), stuck

    def sb(self, shape, dt, name=None):
        self.n_sb += 1
        return self.nc.alloc_sbuf_tensor("sb_" + (name or str(self.n_sb)), list(shape), dt)

    def ps(self, name, shape=(128, 512), dt=F32):
        return self.nc.alloc_psum_tensor("ps_" + name, list(shape), dt)

    def _semh(self, key):
        if key in self.sem:
            return self.sem[key]
        return self.lanes[key][0]

    def _need(self, eng, dep):
        key, val = dep
        if self.seen[eng].get(key, 0) >= val:
            return
        self.seen[eng][key] = val
        self.trace[eng].append(("wait", key, val))
        self.E[eng].wait_ge(self._semh(key), val)

    def _deps(self, eng, reads, writes):
        deps = []
        for r in reads:
            st = self.res.get(r)
            if st and st["w"]:
                deps.append(st["w"])
        for w in writes:
            st = self.res.get(w)
            if st:
                if st["w"]:
                    deps.append(st["w"])
                deps.extend(st["r"].items())
        for d in deps:
            if eng == "pe" and d[0] == "pe":
                continue
            self._need(eng, d)

    def _record(self, me, reads, writes):
        for r in reads:
            st = self.res.setdefault(r, {"w": None, "r": {}})
            st["r"][me[0]] = max(st["r"].get(me[0], 0), me[1])
        for w in writes:
            self.res[w] = {"w": me, "r": {}}

    def op(self, eng, fn, reads=(), writes=()):
        self._deps(eng, reads, writes)
        inst = fn(self.E[eng])
        self.cnt[eng] += 1
        inst.then_inc(self.sem[eng], 1)
        self.trace[eng].append(("inc", eng, 1))
        self._record((eng, self.cnt[eng]), reads, writes)

    def dma(self, q, out, in_, reads=(), writes=(), lane=None, **kw):
        if lane is None:
            lane = "L_" + str(writes[0] if writes else reads[0])
        if lane not in self.lanes:
            self.lanes[lane] = [self.nc.alloc_semaphore("l%d" % len(self.lanes)), 0]
        self._deps(q, reads, writes)
        L = self.lanes[lane]
        L[1] += 1
        self.E[q].dma_start(out=out, in_=in_, **kw).then_inc(L[0], 16)
        self.trace[q].append(("inc", lane, 16))
        self._record((lane, 16 * L[1]), reads, writes)

    def finish(self):
        for lane, (h, c) in self.lanes.items():
            if c:
                self._need("sp", (lane, 16 * c))
        for k in self.sem:
            if self.cnt[k]:
                self._need("sp", (k, self.cnt[k]))


def make_ident(fw, dt=BF16):
    ident = fw.sb([128, 128], dt, "ident")
    fw.op("pool", lambda e: e.memset(ident[:], 1.0), writes=["ident"])
    fw.op("pool", lambda e: e.affine_select(out=ident[:], in_=ident[:], pattern=[[-1, 128]],
                                            compare_op=ALU.is_equal, fill=0.0, base=0,
                                            channel_multiplier=1), reads=["ident"], writes=["ident"])
    return ident


class Rot:
    def __init__(self, items):
        self.items = items
        self.i = 0

    def next(self):
        it = self.items[self.i % len(self.items)]
        self.i += 1
        return it


def rmsnorm_tile(fw, xt, xkey, lnw, out_bf, okey, scr, skey, stat, stkey):
    fw.op("act", lambda e: e.activation(out=scr, in_=xt, func=AF.Square, accum_out=stat[:, 0:1]),
          reads=[xkey], writes=[skey, stkey])
    fw.op("act", lambda e: e.activation(out=stat[:, 1:2], in_=stat[:, 0:1], func=AF.Sqrt, scale=1.0 / D, bias=EPS),
          reads=[stkey], writes=[stkey])
    fw.op("dve", lambda e: e.reciprocal(out=stat[:, 1:2], in_=stat[:, 1:2]), reads=[stkey], writes=[stkey])
    fw.op("dve", lambda e: e.scalar_tensor_tensor(out=out_bf, in0=xt, scalar=stat[:, 1:2], in1=lnw,
                                                  op0=ALU.mult, op1=ALU.mult),
          reads=[xkey, stkey, "lnw"], writes=[okey])


def transpose_cols(fw, src, skey, ncol_blocks, dst_fn, dkeys, tp_rot, ident, evac):
    j = 0
    while j < ncol_blocks:
        n = min(4, ncol_blocks - j)
        tp, tkey = tp_rot.next()
        for i in range(n):
            fw.op("pe", lambda e, i=i, j=j: e.transpose(out=tp[:, i * 128:(i + 1) * 128],
                                                        in_=src[:, (j + i) * 128:(j + i + 1) * 128],
                                                        identity=ident[:]),
                  reads=[skey], writes=[tkey])
        eng = evac.next()
        dst = dst_fn(j, n)
        if eng == "act":
            fw.op("act", lambda e: e.activation(out=dst, in_=tp[:, 0:n * 128].rearrange("p (a b) -> p a b", b=128),
                                                func=AF.Copy), reads=[tkey], writes=dkeys)
        else:
            fw.op("dve", lambda e: e.tensor_copy(out=dst, in_=tp[:, 0:n * 128].rearrange("p (a b) -> p a b", b=128)),
                  reads=[tkey], writes=dkeys)
        j += n


def rope_inplace(fw, sb, sbkey, H, Dh, hf, cos, sin, tmp, tkey):
    v = sb.rearrange("p (h d) -> p h d", d=Dh)
    x1 = v[:, :, 0:hf]
    x2 = v[:, :, hf:2 * hf]
    cb = cos.unsqueeze(1).to_broadcast([128, H, hf])
    sbb = sin.unsqueeze(1).to_broadcast([128, H, hf])
    t = [tmp[:, i, 0:H * hf].rearrange("p (h d) -> p h d", d=hf) for i in range(4)]
    fw.op("dve", lambda e: e.tensor_tensor(out=t[0], in0=x1, in1=cb, op=ALU.mult), reads=[sbkey, "cs"], writes=[tkey + "0"])
    fw.op("dve", lambda e: e.tensor_tensor(out=t[1], in0=x2, in1=sbb, op=ALU.mult), reads=[sbkey, "cs"], writes=[tkey + "1"])
    fw.op("dve", lambda e: e.tensor_tensor(out=t[2], in0=x2, in1=cb, op=ALU.mult), reads=[sbkey, "cs"], writes=[tkey + "2"])
    fw.op("dve", lambda e: e.tensor_tensor(out=t[3], in0=x1, in1=sbb, op=ALU.mult), reads=[sbkey, "cs"], writes=[tkey + "3"])
    fw.op("dve", lambda e: e.tensor_tensor(out=x1, in0=t[0], in1=t[1], op=ALU.subtract),
          reads=[tkey + "0", tkey + "1"], writes=[sbkey])
    fw.op("dve", lambda e: e.tensor_tensor(out=x2, in0=t[2], in1=t[3], op=ALU.add),
          reads=[tkey + "2", tkey + "3"], writes=[sbkey])


def build_cos_sin(fw, pos_d, invf_d, nfreq):
    posi = fw.sb([128, NT], I32, "posi")
    posf = fw.sb([128, NT], F32, "posf")
    invf = fw.sb([128, nfreq], F32, "invf")
    ang = fw.sb([128, NT, nfreq], F32, "ang")
    kf = fw.sb([128, NT, nfreq], F32, "kf")
    cos = fw.sb([128, NT, nfreq], F32, "cos")
    sin = fw.sb([128, NT, nfreq], F32, "sin")
    fw.dma("sp", posi[:], pos_d, writes=["posi"])
    fw.dma("sp", invf[:], invf_d, writes=["invf"])
    fw.op("dve", lambda e: e.tensor_copy(out=posf[:], in_=posi[:]), reads=["posi"], writes=["posf"])
    for t in range(NT):
        fw.op("dve", lambda e, t=t: e.tensor_scalar(out=ang[:, t, :], in0=invf[:], scalar1=posf[:, t:t + 1],
                                                    scalar2=None, op0=ALU.mult), reads=["posf", "invf"], writes=["ang"])
    MAGIC = 12582912.0
    fw.op("dve", lambda e: e.tensor_scalar(out=kf[:], in0=ang[:], scalar1=1.0 / TWO_PI, scalar2=MAGIC,
                                           op0=ALU.mult, op1=ALU.add), reads=["ang"], writes=["kf"])
    fw.op("dve", lambda e: e.tensor_scalar(out=kf[:], in0=kf[:], scalar1=MAGIC, scalar2=None, op0=ALU.subtract),
          reads=["kf"], writes=["kf"])
    fw.op("dve", lambda e: e.scalar_tensor_tensor(out=ang[:], in0=kf[:], scalar=-TWO_PI, in1=ang[:],
                                                  op0=ALU.mult, op1=ALU.add), reads=["kf", "ang"], writes=["ang"])
    PI_S = 3.1415925
    fw.op("dve", lambda e: e.tensor_scalar(out=ang[:], in0=ang[:], scalar1=PI_S, scalar2=-PI_S, op0=ALU.min, op1=ALU.max),
          reads=["ang"], writes=["ang"])
    fw.op("act", lambda e: e.activation(out=sin[:], in_=ang[:], func=AF.Sin), reads=["ang"], writes=["cs"])
    fw.op("act", lambda e: e.activation(out=kf[:], in_=ang[:], func=AF.Sin, scale=0.5), reads=["ang"], writes=["kf"])
    fw.op("dve", lambda e: e.tensor_tensor(out=kf[:], in0=kf[:], in1=kf[:], op=ALU.mult), reads=["kf"], writes=["kf"])
    fw.op("dve", lambda e: e.tensor_scalar(out=cos[:], in0=kf[:], scalar1=-2.0, scalar2=1.0, op0=ALU.mult,
                                           op1=ALU.add), reads=["kf", "cs"], writes=["cs"])
    return cos, sin


def mem_kv_setup(fw, mem_d, mnw_d, wkv_d, ident, lnw_scratch, wbufs, acc_rot, tp_rot, evac, scratch=None):
    memKT = fw.sb([128, 4, 256], BF16, "memKT")
    memV = fw.sb([128, 2, 4, 129], BF16, "memV")
    memnT = fw.sb([128, 16, 256], BF16, "memnT")
    if scratch is None:
        mx = fw.sb([128, D], F32, "mem_x")
        mscr = fw.sb([128, D], F32, "mem_scr")
        mh = fw.sb([128, D], BF16, "mem_h")
    else:
        mx, mscr, mh = scratch
    mst = fw.sb([128, 2], F32, "mem_st")
    fw.dma("sp", lnw_scratch[:], mnw_d.partition_broadcast(128), writes=["lnw"])
    for mt in range(2):
        fw.dma("sp", mx[:], mem_d[mt * 128:(mt + 1) * 128, :], writes=["mem_x"])
        rmsnorm_tile(fw, mx[:], "mem_x", lnw_scratch[:], mh[:], "mem_h", mscr[:], "mem_scr", mst, "mem_st")
        transpose_cols(fw, mh, "mem_h", 16,
                       lambda j, n, mt=mt: memnT[:, j:j + n, mt * 128:(mt + 1) * 128], ["memnT"], tp_rot, ident, evac)
    fw.op("pool", lambda e: e.memset(memV[:], 1.0), writes=["memV"])
    wv = wkv_d.rearrange("(k p) n -> p k n", p=128)
    wb, wkey = wbufs.next()
    fw.dma("pool", wb[:, :, 0:512], wv[:, :, 0:512], writes=[wkey])
    for h in range(4):
        acc, akey = acc_rot.next()
        for k in range(16):
            fw.op("pe", lambda e, k=k, h=h: e.matmul(acc[:, 0:256], lhsT=wb[:, k, h * 128:(h + 1) * 128],
                                                     rhs=memnT[:, k, :], start=(k == 0), stop=(k == 15)),
                  reads=[wkey, "memnT"], writes=[akey])
        fw.op("act", lambda e, h=h: e.activation(out=memKT[:, h, :], in_=acc[:, 0:256], func=AF.Copy),
              reads=[akey], writes=["memKT"])
    wb, wkey = wbufs.next()
    fw.dma("pool", wb[:, :, 0:512], wv[:, :, 512:1024], writes=[wkey])
    for mt in range(2):
        acc, akey = acc_rot.next()
        for k in range(16):
            fw.op("pe", lambda e, k=k, mt=mt: e.matmul(acc[:, 0:512], lhsT=memnT[:, k, mt * 128:(mt + 1) * 128],
                                                       rhs=wb[:, k, 0:512], start=(k == 0), stop=(k == 15)),
                  reads=[wkey, "memnT"], writes=[akey])
        fw.op("dve", lambda e, mt=mt: e.tensor_copy(out=memV[:, mt, :, 0:128],
                                                    in_=acc[:, 0:512].rearrange("p (h d) -> p h d", d=128)),
              reads=[akey], writes=["memV"])
    return memKT, memV


def mem_attention(fw, mqT, mqkey, memKT, memV, msA, msB, moA, moB, E, ekey, rden, rkey, out_bf, okey,
                  pk=("msA", "msB", "moA", "moB")):
    scale = 128 ** -0.5
    for h in range(4):
        ms = msA if h < 2 else msB
        mkey = pk[0] if h < 2 else pk[1]
        for mt in range(2):
            c0 = ((h % 2) * 2 + mt) * 128
            fw.op("pe", lambda e, h=h, mt=mt, c0=c0, ms=ms: e.matmul(ms[:, c0:c0 + 128],
                                                                     lhsT=memKT[:, h, mt * 128:(mt + 1) * 128],
                                                                     rhs=mqT[:, h, :], start=True, stop=True),
                  reads=[mqkey, "memKT"], writes=[mkey])
    fw.op("act", lambda e: e.activation(out=E[:, 0:4, :], in_=msA[:, :].rearrange("p (a b) -> p a b", b=128),
                                        func=AF.Exp, scale=scale), reads=[pk[0]], writes=[ekey + "A"])
    fw.op("act", lambda e: e.activation(out=E[:, 4:8, :], in_=msB[:, :].rearrange("p (a b) -> p a b", b=128),
                                        func=AF.Exp, scale=scale), reads=[pk[1]], writes=[ekey + "B"])
    for h in range(4):
        mo = moA if h < 2 else moB
        mokey = pk[2] if h < 2 else pk[3]
        for mt in range(2):
            c0 = (h % 2) * 129
            fw.op("pe", lambda e, h=h, mt=mt, c0=c0, mo=mo: e.matmul(mo[:, c0:c0 + 129], lhsT=E[:, h * 2 + mt, :],
                                                                     rhs=memV[:, mt, h, :], start=(mt == 0),
                                                                     stop=(mt == 1)),
                  reads=[ekey + ("A" if h < 2 else "B"), "memV"], writes=[mokey])
    for half, (mo, mokey) in enumerate(((moA, pk[2]), (moB, pk[3]))):
        mv = mo[:, 0:258].rearrange("p (h d) -> p h d", d=129)
        fw.op("dve", lambda e, mv=mv, half=half: e.reciprocal(out=rden[:, half * 2:half * 2 + 2], in_=mv[:, :, 128]),
              reads=[mokey], writes=[rkey + str(half)])
        fw.op("dve", lambda e, mv=mv, half=half: e.tensor_tensor(
            out=out_bf[:, half * 256:(half + 1) * 256].rearrange("p (h d) -> p h d", d=128),
            in0=mv[:, :, 0:128], in1=rden[:, half * 2:half * 2 + 2].unsqueeze(2).to_broadcast([128, 2, 128]),
            op=ALU.mult), reads=[mokey, rkey + str(half)], writes=[okey])


def build_proj0():
    nc = bass.Bass("TRN2", target_bir_lowering=False)
    fw = FW(nc)
    x_d = nc.dram_tensor("x", [T, D], F32, kind="ExternalInput").ap()
    pos_d = nc.dram_tensor("pos", [128, NT], I32, kind="ExternalInput").ap()
    invf_d = nc.dram_tensor("invf", [128, 24], F32, kind="ExternalInput").ap()
    lnw_d = nc.dram_tensor("lnw", [D], F32, kind="ExternalInput").ap()
    w_d = nc.dram_tensor("w_in", [D, DSA_IN], F32, kind="ExternalInput").ap()
    mem_d = nc.dram_tensor("mem", [256, D], F32, kind="ExternalInput").ap()
    mnw_d = nc.dram_tensor("mnw", [D], F32, kind="ExternalInput").ap()
    wkv_d = nc.dram_tensor("wkv", [D, 1024], F32, kind="ExternalInput").ap()
    qT_d = nc.dram_tensor("qT", [12, 128, T], BF16, kind="ExternalOutput").ap()
    kT_d = nc.dram_tensor("kT", [12, 128, T], BF16, kind="ExternalOutput").ap()
    v_d = nc.dram_tensor("v", [T, MW], BF16, kind="ExternalOutput").ap()
    iqT_d = nc.dram_tensor("iqT", [8, 128, T], BF16, kind="ExternalOutput").ap()
    ikT_d = nc.dram_tensor("ikT", [128, T], BF16, kind="ExternalOutput").ap()
    iw_d = nc.dram_tensor("iw", [T, 16], F32, kind="ExternalOutput").ap()
    mo_d = nc.dram_tensor("memout", [T, 512], BF16, kind="ExternalOutput").ap()

    ident = make_ident(fw)
    lnw = fw.sb([128, D], F32, "lnw")
    hT = fw.sb([128, 16, T], BF16, "hT")
    wb0 = fw.sb([128, 16, 512], BF16, "wb0")
    wb1 = fw.sb([128, 16, 512], BF16, "wb1")
    wbufs = Rot([(wb0, "wb0"), (wb1, "wb1")])
    accs = Rot([(fw.ps("acc0"), "acc0"), (fw.ps("acc1"), "acc1")])
    tps = Rot([(fw.ps("tp0", (128, 512), BF16), "tp0"), (fw.ps("tp1", (128, 512), BF16), "tp1")])
    msA, msB, moA, moB = fw.ps("msA"), fw.ps("msB"), fw.ps("moA"), fw.ps("moB")
    evac = Rot(["act", "dve"])

    cos, sin = build_cos_sin(fw, pos_d, invf_d, 24)
    memKT, memV = mem_kv_setup(fw, mem_d, mnw_d, wkv_d, ident, lnw, wbufs, accs, tps, evac)

    fw.dma("sp", lnw[:], lnw_d.partition_broadcast(128), reads=[], writes=["lnw"])
    xts = Rot([(fw.sb([128, D], F32, "xt0"), "xt0"), (fw.sb([128, D], F32, "xt1"), "xt1")])
    hbs = Rot([(fw.sb([128, D], BF16, "hb0"), "hb0"), (fw.sb([128, D], BF16, "hb1"), "hb1")])
    scr = fw.sb([128, D], F32, "scr")
    stat = fw.sb([128, 2], F32, "stat")
    for t in range(NT):
        xt, xkey = xts.next()
        hb, hkey = hbs.next()
        fw.dma("sp", xt[:], x_d[t * 128:(t + 1) * 128, :], writes=[xkey])
        rmsnorm_tile(fw, xt[:], xkey, lnw[:], hb[:], hkey, scr[:], "scr", stat, "stat")
        transpose_cols(fw, hb, hkey, 16, lambda j, n, t=t: hT[:, j:j + n, t * 128:(t + 1) * 128],
                       [("hT", t)], tps, ident, evac)

    wv = w_d.rearrange("(k p) n -> p k n", p=128)
    blocks = []
    for i in range(3):
        blocks.append(("q", i, i * 512, 512))
    for i in range(3):
        blocks.append(("k", i, MW + i * 512, 512))
    for i in range(3):
        blocks.append(("v", i, 2 * MW + i * 512, 512))
    for i in range(2):
        blocks.append(("iq", i, 3 * MW + i * 512, 512))
    blocks.append(("tail", 0, 3 * MW + 1024, 80))
    blocks.append(("mq", 0, 3 * MW + 1024 + 80, 512))

    sbs = Rot([(fw.sb([128, 512], F32, "pp0"), "pp0"), (fw.sb([128, 512], F32, "pp1"), "pp1")])
    pbs = Rot([(fw.sb([128, 512], BF16, "pb0"), "pb0"), (fw.sb([128, 512], BF16, "pb1"), "pb1")])
    rtmp = fw.sb([128, 4, 128], F32, "rtmp")
    stg = Rot([(fw.sb([128, 4, 512], BF16, "stg0"), "stg0"), (fw.sb([128, 4, 512], BF16, "stg1"), "stg1")])
    vst = Rot([(fw.sb([128, 512], BF16, "vst0"), "vst0"), (fw.sb([128, 512], BF16, "vst1"), "vst1")])
    iwst = Rot([(fw.sb([128, 16], F32, "iwst0"), "iwst0"), (fw.sb([128, 16], F32, "iwst1"), "iwst1")])
    ikst = Rot([(fw.sb([128, 512], BF16, "ikst0"), "ikst0"), (fw.sb([128, 512], BF16, "ikst1"), "ikst1")])
    mqT = fw.sb([128, 4, 128], BF16, "mqT")
    E = fw.sb([128, 8, 128], BF16, "memE")
    rden = fw.sb([128, 4], F32, "rden")
    most = Rot([(fw.sb([128, 512], BF16, "most0"), "most0"), (fw.sb([128, 512], BF16, "most1"), "most1")])

    nxt = wbufs.next()
    fw.dma("pool", nxt[0][:, :, 0:blocks[0][3]], wv[:, :, blocks[0][2]:blocks[0][2] + blocks[0][3]], writes=[nxt[1]])
    for bidx, (kind, bi, c0, ncol) in enumerate(blocks):
        wb, wkey = nxt
        if bidx + 1 < len(blocks):
            nxt = wbufs.next()
            nb = blocks[bidx + 1]
            fw.dma("pool", nxt[0][:, :, 0:nb[3]], wv[:, :, nb[2]:nb[2] + nb[3]], writes=[nxt[1]])
        for g in range(NT // 4):
            if kind in ("q", "k", "iq"):
                sg, sgkey = stg.next()
            if kind == "tail":
                ikg, ikgkey = ikst.next()
            for tt in range(4):
                t = g * 4 + tt
                acc, akey = accs.next()
                for k in range(16):
                    fw.op("pe", lambda e, k=k, t=t: e.matmul(acc[:, 0:ncol], lhsT=hT[:, k, t * 128:(t + 1) * 128],
                                                             rhs=wb[:, k, 0:ncol], start=(k == 0), stop=(k == 15)),
                          reads=[wkey, ("hT", t)], writes=[akey])
                if kind == "v":
                    vs, vkey = vst.next()
                    fw.op("act", lambda e: e.activation(out=vs[:], in_=acc[:], func=AF.Copy), reads=[akey], writes=[vkey])
                    fw.dma("pool", v_d[t * 128:(t + 1) * 128, bi * 512:(bi + 1) * 512], vs[:], reads=[vkey],
                           lane="st_" + vkey)
                    continue
                pp, pkey = sbs.next()
                fw.op("act", lambda e: e.activation(out=pp[:, 0:ncol], in_=acc[:, 0:ncol], func=AF.Copy),
                      reads=[akey], writes=[pkey])
                pb, pbkey = pbs.next()
                if kind in ("q", "k"):
                    rope_inplace(fw, pp[:], pkey, 4, 128, 16, cos[:, t, 0:16], sin[:, t, 0:16], rtmp, "rtmp")
                    fw.op("dve", lambda e: e.tensor_copy(out=pb[:], in_=pp[:]), reads=[pkey], writes=[pbkey])
                    transpose_cols(fw, pb, pbkey, 4, lambda j, n, tt=tt: sg[:, j:j + n, tt * 128:(tt + 1) * 128],
                                   [sgkey], tps, ident, evac)
                elif kind == "iq":
                    rope_inplace(fw, pp[:], pkey, 8, 64, 8, cos[:, t, 16:24], sin[:, t, 16:24], rtmp, "rtmp")
                    fw.op("dve", lambda e: e.tensor_copy(out=pb[:], in_=pp[:]), reads=[pkey], writes=[pbkey])
                    transpose_cols(fw, pb, pbkey, 4, lambda j, n, tt=tt: sg[:, j:j + n, tt * 128:(tt + 1) * 128],
                                   [sgkey], tps, ident, evac)
                elif kind == "tail":
                    rope_inplace(fw, pp[:, 0:64], pkey, 1, 64, 8, cos[:, t, 16:24], sin[:, t, 16:24], rtmp, "rtmp")
                    fw.op("dve", lambda e: e.tensor_copy(out=pb[:, 0:64], in_=pp[:, 0:64]), reads=[pkey], writes=[pbkey])
                    fw.op("dve", lambda e: e.tensor_copy(out=pb[:, 64:128], in_=pp[:, 0:64]), reads=[pkey], writes=[pbkey])
                    iws, iwkey = iwst.next()
                    fw.op("dve", lambda e: e.tensor_scalar(out=iws[:], in0=pp[:, 64:80], scalar1=1.0 / 32.0, scalar2=None,
                                                           op0=ALU.mult), reads=[pkey], writes=[iwkey])
                    fw.dma("pool", iw_d[t * 128:(t + 1) * 128, :], iws[:], reads=[iwkey], lane="st_" + iwkey)
                    transpose_cols(fw, pb, pbkey, 1,
                                   lambda j, n, tt=tt: ikg[:, tt * 128:(tt + 1) * 128].rearrange("p (a b) -> p a b", a=1),
                                   [ikgkey], tps, ident, evac)
                elif kind == "mq":
                    fw.op("dve", lambda e: e.tensor_copy(out=pb[:], in_=pp[:]), reads=[pkey], writes=[pbkey])
                    transpose_cols(fw, pb, pbkey, 4, lambda j, n: mqT[:, j:j + n, :], ["mqT"], tps, ident, evac)
                    mo, mokey = most.next()
                    mem_attention(fw, mqT, "mqT", memKT, memV, msA, msB, moA, moB, E, "memE", rden, "rden", mo, mokey)
                    fw.dma("pool", mo_d[t * 128:(t + 1) * 128, :], mo[:], reads=[mokey], lane="st_" + mokey)
            tok = slice(g * 512, (g + 1) * 512)
            if kind == "q":
                fw.dma("pool", qT_d[bi * 4:(bi + 1) * 4, :, tok].rearrange("h d t -> d h t"), sg[:], reads=[sgkey],
                       lane="st_" + sgkey)
            elif kind == "k":
                fw.dma("pool", kT_d[bi * 4:(bi + 1) * 4, :, tok].rearrange("h d t -> d h t"), sg[:], reads=[sgkey],
                       lane="st_" + sgkey)
            elif kind == "iq":
                fw.dma("pool", iqT_d[bi * 4:(bi + 1) * 4, :, tok].rearrange("h d t -> d h t"), sg[:], reads=[sgkey],
                       lane="st_" + sgkey)
            elif kind == "tail":
                fw.dma("pool", ikT_d[:, tok], ikg[:], reads=[ikgkey], lane="st_" + ikgkey)
    fw.finish()
    return nc


def rope_invf():
    f16 = (500000.0 ** (-np.arange(16, dtype=np.float32) / np.float32(16))).astype(np.float32)
    f8 = (500000.0 ** (-np.arange(8, dtype=np.float32) / np.float32(8))).astype(np.float32)
    return np.tile(np.concatenate([f16, f8])[None, :], (128, 1)).astype(np.float32)


NIT = 18
TOPK = 256


def build_attn0():
    nc = bass.Bass("TRN2", target_bir_lowering=False)
    fw = FW(nc)
    qT_d = nc.dram_tensor("qT", [12, 128, T], BF16, kind="ExternalInput").ap()
    iqT_d = nc.dram_tensor("iqT", [8, 128, T], BF16, kind="ExternalInput").ap()
    iw_d = nc.dram_tensor("iw", [T, 16], F32, kind="ExternalInput").ap()
    kT_d = nc.dram_tensor("kT", [12, 128, SEQ], BF16, kind="ExternalInput").ap()
    v_d = nc.dram_tensor("v", [SEQ, MW], BF16, kind="ExternalInput").ap()
    ikT_d = nc.dram_tensor("ikT", [128, SEQ], BF16, kind="ExternalInput").ap()
    cmask_d = nc.dram_tensor("cmask", [128, 1024], F32, kind="ExternalInput").ap()
    ctab_d = nc.dram_tensor("ctab", [128, 32], F32, kind="ExternalInput").ap()
    mix_d = nc.dram_tensor("mix", [T, MW], BF16, kind="ExternalOutput").ap()
    attn_phase(nc, fw, qT_d, iqT_d, iw_d, kT_d, v_d, ikT_d, cmask_d, ctab_d, mix_d)
    fw.finish()
    return nc


def attn_phase(nc, fw, qT_d, iqT_d, iw_d, kT_d, v_d, ikT_d, cmask_d, ctab_d, mix_d, nrounds=NT, dbg=99):
    ident = make_ident(fw)
    ones = fw.sb([128, 1], BF16, "ones")
    fw.op("pool", lambda e: e.memset(ones[:], 1.0), writes=["ones"])
    ikT = fw.sb([128, SEQ], BF16, "ikT")
    for i in range(4):
        fw.dma("sp", ikT[:, i * 4096:(i + 1) * 4096], ikT_d[:, i * 4096:(i + 1) * 4096], writes=[("ikT", i)])
    cmask = fw.sb([128, 1024], F32, "cmask")
    fw.dma("sp", cmask[:], cmask_d, writes=["cmask"])
    ctab = fw.sb([128, 32], F32, "ctab")
    fw.dma("sp", ctab[:], ctab_d, writes=["ctab"])
    sc = fw.sb([128, SEQ], F32, "sc")
    m01 = fw.sb([128, SEQ], BF16, "m01")
    qTs = Rot([(fw.sb([128, 12, 128], BF16, "qTb%d" % i), "qTb%d" % i) for i in range(2)])
    iqTs = Rot([(fw.sb([128, 8, 128], BF16, "iqTb%d" % i), "iqTb%d" % i) for i in range(2)])
    iws = Rot([(fw.sb([128, 16], F32, "iwb%d" % i), "iwb%d" % i) for i in range(2)])
    rls = Rot([(fw.sb([128, 512], F32, "rl%d" % i), "rl%d" % i) for i in range(3)])
    kTcs = Rot([(fw.sb([128, 12, 256], BF16, "kTc%d" % i), "kTc%d" % i) for i in range(2)])
    vcs = Rot([(fw.sb([128, 2, MW], BF16, "vc%d" % i), "vc%d" % i) for i in range(2)])
    mTs = Rot([(fw.sb([128, 2, 128], BF16, "mTc%d" % i), "mTc%d" % i) for i in range(2)])
    Es = Rot([(fw.sb([128, 4, 128], BF16, "E%d" % i), "E%d" % i) for i in range(3)])
    PTs = Rot([(fw.sb([128, 4, 128], BF16, "PT%d" % i), "PT%d" % i) for i in range(3)])
    osts = Rot([(fw.sb([128, MW], BF16, "ost%d" % i), "ost%d" % i) for i in range(2)])
    st8 = fw.sb([128, 8], F32, "st8")
    lo = fw.sb([128, 1], F32, "lo")
    mid = fw.sb([128, 1], F32, "mid")
    cnt = fw.sb([128, 1], F32, "cnt")
    tt = fw.sb([128, 1], F32, "tt")
    dtab = fw.sb([128, 32], F32, "dtab")
    rden = fw.sb([128, 12], F32, "rden")
    O = [(fw.ps("O%d" % i), "O%d" % i) for i in range(3)]
    sts = Rot([(fw.ps("st%d" % i), "st%d" % i) for i in range(2)])
    idxs = Rot([(fw.ps("ix%d" % i), "ix%d" % i) for i in range(1)])
    den = fw.ps("den", (128, 16), F32)
    tp = fw.ps("tpm", (128, 256), BF16)
    scale = 128 ** -0.5

    for j in range(nrounds):
        n = 1024 * (j + 1)
        nch = n // 512
        qTb, qkey = qTs.next()
        iqTb, iqkey = iqTs.next()
        iwb, iwkey = iws.next()
        tok = slice(j * 128, (j + 1) * 128)
        fw.dma("sp", qTb[:], qT_d[:, :, tok].rearrange("h d t -> d h t"), writes=[qkey])
        fw.dma("sp", iqTb[:], iqT_d[:, :, tok].rearrange("h d t -> d h t"), writes=[iqkey])
        fw.dma("sp", iwb[:], iw_d[tok, :], writes=[iwkey])
        for ch in range(nch):
            cs = slice(ch * 512, (ch + 1) * 512)
            sckey = ("sc", ch)
            for h in range(16):
                ix, ixkey = idxs.next()
                r0 = (h % 2) * 64
                fw.op("pe", lambda e, h=h, r0=r0, ix=ix: e.matmul(ix[:, :], lhsT=iqTb[r0:r0 + 64, h // 2, :],
                                                                 rhs=ikT[r0:r0 + 64, cs], start=True, stop=True),
                      reads=[iqkey, ("ikT", ch // 8)], writes=[ixkey])
                rl, rlkey = rls.next()
                fw.op("act", lambda e, ix=ix, rl=rl: e.activation(out=rl[:], in_=ix[:], func=AF.Relu),
                      reads=[ixkey], writes=[rlkey])
                if h == 0:
                    fw.op("dve", lambda e, rl=rl: e.tensor_scalar(out=sc[:, cs], in0=rl[:], scalar1=iwb[:, 0:1],
                                                                  scalar2=None, op0=ALU.mult),
                          reads=[rlkey, iwkey, ("m01", ch)], writes=[sckey])
                else:
                    fw.op("dve", lambda e, rl=rl, h=h: e.scalar_tensor_tensor(out=sc[:, cs], in0=rl[:],
                                                                              scalar=iwb[:, h:h + 1], in1=sc[:, cs],
                                                                              op0=ALU.mult, op1=ALU.add),
                          reads=[rlkey, iwkey, sckey], writes=[sckey])
        allsc = [("sc", ch) for ch in range(nch)]
        if dbg < 1:
            continue
        fw.op("dve", lambda e: e.tensor_reduce(out=lo[:], in_=sc[:, 0:n], axis=AX.X, op=ALU.min),
              reads=allsc, writes=["lo"])
        if dbg < 1.2:
            continue
        fw.op("dve", lambda e: e.tensor_tensor(out=sc[:, n - 1024:n], in0=sc[:, n - 1024:n], in1=cmask[:], op=ALU.add),
              reads=[("sc", nch - 2), ("sc", nch - 1), "cmask", "lo"], writes=[("sc", nch - 2), ("sc", nch - 1)])
        fw.op("dve", lambda e: e.tensor_reduce(out=st8[:, 0:1], in_=sc[:, 0:n], axis=AX.X, op=ALU.max),
              reads=allsc, writes=["st8"])
        fw.op("dve", lambda e: e.tensor_tensor(out=tt[:], in0=st8[:, 0:1], in1=lo[:], op=ALU.subtract),
              reads=["st8", "lo"], writes=["tt"])
        fw.op("dve", lambda e: e.tensor_scalar(out=dtab[:], in0=ctab[:], scalar1=tt[:, 0:1], scalar2=None, op0=ALU.mult),
              reads=["tt", "ctab"], writes=["dtab"])
        if dbg < 1.4:
            continue
        for it in range(NIT):
            fw.op("dve", lambda e, it=it: e.tensor_tensor(out=mid[:], in0=lo[:], in1=dtab[:, it:it + 1], op=ALU.add),
                  reads=["lo", "dtab"], writes=["mid"])
            fw.op("dve", lambda e: e.tensor_scalar(out=m01[:, 0:n], in0=sc[:, 0:n], scalar1=mid[:, 0:1], scalar2=None,
                                                   op0=ALU.is_ge, op1=ALU.add, accum_out=cnt[:]),
                  reads=allsc + ["mid"], writes=["m01all", "cnt"])
            fw.op("dve", lambda e: e.tensor_scalar(out=tt[:], in0=cnt[:], scalar1=TOPK - 0.5, scalar2=None,
                                                   op0=ALU.is_ge), reads=["cnt"], writes=["tt"])
            fw.op("dve", lambda e, it=it: e.tensor_tensor(out=tt[:], in0=tt[:], in1=dtab[:, it:it + 1], op=ALU.mult),
                  reads=["tt", "dtab"], writes=["tt"])
            fw.op("dve", lambda e: e.tensor_tensor(out=lo[:], in0=lo[:], in1=tt[:], op=ALU.add),
                  reads=["lo", "tt"], writes=["lo"])
        if dbg < 1.6:
            continue
        for ch in range(nch):
            cs = slice(ch * 512, (ch + 1) * 512)
            fw.op("dve", lambda e, cs=cs: e.tensor_scalar(out=m01[:, cs], in0=sc[:, cs], scalar1=lo[:, 0:1], scalar2=None,
                                                          op0=ALU.is_ge), reads=[("sc", ch), "lo", "m01all"],
                  writes=[("m01", ch)])
        if dbg < 2:
            continue
        nkc = n // 256

        def load_kv(kc):
            kTc, kkey = kTcs.next()
            vc, vkey = vcs.next()
            ks = slice(kc * 256, (kc + 1) * 256)
            fw.dma("sp", kTc[:], kT_d[:, :, ks].rearrange("h d t -> d h t"), writes=[kkey])
            fw.dma("sp", vc[:], v_d[ks, :].rearrange("(b p) f -> p b f", p=128), writes=[vkey])
            return kTc, kkey, vc, vkey

        nxt = load_kv(0)
        for kc in range(nkc):
            kTc, kkey, vc, vkey = nxt
            if kc + 1 < nkc:
                nxt = load_kv(kc + 1)
            mTc, mkey = mTs.next()
            for b in range(2):
                kb = kc * 2 + b
                fw.op("pe", lambda e, b=b, kb=kb: e.transpose(out=tp[:, b * 128:(b + 1) * 128],
                                                              in_=m01[:, kb * 128:(kb + 1) * 128], identity=ident[:]),
                      reads=[("m01", kb // 4)], writes=["tpm"])
            fw.op("dve", lambda e: e.tensor_copy(out=mTc[:], in_=tp[:, :].rearrange("p (a b) -> p a b", b=128)),
                  reads=["tpm"], writes=[mkey])
            for b in range(2):
                first = (kc == 0 and b == 0)
                last = (kc == nkc - 1 and b == 1)
                for hg in range(3):
                    st, stkey = sts.next()
                    for hh in range(4):
                        h = hg * 4 + hh
                        fw.op("pe", lambda e, h=h, hh=hh, b=b, st=st: e.matmul(
                            st[:, hh * 128:(hh + 1) * 128], lhsT=kTc[:, h, b * 128:(b + 1) * 128], rhs=qTb[:, h, :],
                            start=True, stop=True), reads=[kkey, qkey], writes=[stkey])
                    E, ekey = Es.next()
                    fw.op("act", lambda e, st=st, E=E: e.activation(
                        out=E[:], in_=st[:, :].rearrange("p (a b) -> p a b", b=128), func=AF.Exp, scale=scale),
                        reads=[stkey], writes=[ekey])
                    PT, pkey = PTs.next()
                    fw.op("dve", lambda e, E=E, PT=PT, b=b: e.tensor_tensor(
                        out=PT[:], in0=E[:], in1=mTc[:, b, :].unsqueeze(1).to_broadcast([128, 4, 128]), op=ALU.mult),
                        reads=[ekey, mkey], writes=[pkey])
                    for hh in range(4):
                        h = hg * 4 + hh
                        Ob, okey = O[h // 4]
                        fw.op("pe", lambda e, h=h, hh=hh, b=b, PT=PT, Ob=Ob: e.matmul(
                            Ob[:, (h % 4) * 128:(h % 4 + 1) * 128], lhsT=PT[:, hh, :],
                            rhs=vc[:, b, h * 128:(h + 1) * 128], start=(first and h % 4 == 0), stop=last),
                            reads=[pkey, vkey], writes=[okey])
                        fw.op("pe", lambda e, h=h, hh=hh, PT=PT: e.matmul(
                            den[:, h:h + 1], lhsT=PT[:, hh, :], rhs=ones[:, 0:1], start=(first and h == 0), stop=last),
                            reads=[pkey, "ones"], writes=["den"])
        ost, ostkey = osts.next()
        fw.op("dve", lambda e: e.reciprocal(out=rden[:], in_=den[:, 0:12]), reads=["den"], writes=["rden"])
        for g in range(3):
            Ob, okey = O[g]
            fw.op("dve", lambda e, g=g, Ob=Ob: e.tensor_tensor(
                out=ost[:, g * 512:(g + 1) * 512].rearrange("p (h d) -> p h d", d=128),
                in0=Ob[:, :].rearrange("p (h d) -> p h d", d=128),
                in1=rden[:, g * 4:(g + 1) * 4].unsqueeze(2).to_broadcast([128, 4, 128]), op=ALU.mult),
                reads=[okey, "rden"], writes=[ostkey])
        fw.dma("pool", mix_d[tok, :], ost[:], reads=[ostkey], lane="st_" + ostkey)


def build_outproj():
    nc = bass.Bass("TRN2", target_bir_lowering=False)
    fw = FW(nc)
    x_d = nc.dram_tensor("x", [T, D], F32, kind="ExternalInput").ap()
    cat_d = nc.dram_tensor("cat", [T, D], BF16, kind="ExternalInput").ap()
    w_d = nc.dram_tensor("w_out", [D, D], F32, kind="ExternalInput").ap()
    y_d = nc.dram_tensor("x1", [T, D], F32, kind="ExternalOutput").ap()
    ident = make_ident(fw)
    catT = fw.sb([128, 16, T], BF16, "catT")
    cts = Rot([(fw.sb([128, D], BF16, "ct%d" % i), "ct%d" % i) for i in range(2)])
    tps = Rot([(fw.ps("tp%d" % i, (128, 512), BF16), "tp%d" % i) for i in range(2)])
    accs = Rot([(fw.ps("acc%d" % i), "acc%d" % i) for i in range(2)])
    evac = Rot(["act", "dve"])
    for t in range(NT):
        ct, ckey = cts.next()
        fw.dma("sp", ct[:], cat_d[t * 128:(t + 1) * 128, :], writes=[ckey])
        transpose_cols(fw, ct, ckey, 16, lambda j, n, t=t: catT[:, j:j + n, t * 128:(t + 1) * 128],
                       [("catT", t)], tps, ident, evac)
    wv = w_d.rearrange("(k p) n -> p k n", p=128)
    wbs = Rot([(fw.sb([128, 16, 512], BF16, "wb%d" % i), "wb%d" % i) for i in range(2)])
    xs = Rot([(fw.sb([128, 512], F32, "xc%d" % i), "xc%d" % i) for i in range(3)])
    for cb in range(4):
        wb, wkey = wbs.next()
        fw.dma("pool", wb[:], wv[:, :, cb * 512:(cb + 1) * 512], writes=[wkey])
        for t in range(NT):
            xc, xkey = xs.next()
            fw.dma("sp", xc[:], x_d[t * 128:(t + 1) * 128, cb * 512:(cb + 1) * 512], writes=[xkey])
            acc, akey = accs.next()
            for k in range(16):
                fw.op("pe", lambda e, k=k, t=t: e.matmul(acc[:], lhsT=catT[:, k, t * 128:(t + 1) * 128], rhs=wb[:, k, :],
                                                         start=(k == 0), stop=(k == 15)),
                      reads=[wkey, ("catT", t)], writes=[akey])
            fw.op("dve", lambda e, xc=xc, acc=acc: e.tensor_tensor(out=xc[:], in0=xc[:], in1=acc[:], op=ALU.add),
                  reads=[xkey, akey], writes=[xkey])
            fw.dma("sp", y_d[t * 128:(t + 1) * 128, cb * 512:(cb + 1) * 512], xc[:], reads=[xkey], lane="st_" + xkey)
    fw.finish()
    return nc


def build_ffn(final):
    nc = bass.Bass("TRN2", target_bir_lowering=False)
    fw = FW(nc)
    x_d = nc.dram_tensor("x1", [T, D], F32, kind="ExternalInput").ap()
    xh_d = nc.dram_tensor("x1h", [128, D], F32, kind="ExternalInput").ap()
    lnw_d = nc.dram_tensor("lnw", [D], F32, kind="ExternalInput").ap()
    fnw_d = nc.dram_tensor("fnw", [D], F32, kind="ExternalInput").ap()
    wg_d = nc.dram_tensor("wg", [D, DFF], F32, kind="ExternalInput").ap()
    wu_d = nc.dram_tensor("wu", [D, DFF], F32, kind="ExternalInput").ap()
    wd_d = nc.dram_tensor("wd", [DFF, D], F32, kind="ExternalInput").ap()
    cw_d = nc.dram_tensor("cw", [128, NFB, 3], F32, kind="ExternalInput").ap()
    cb_d = nc.dram_tensor("cb", [128, NFB], F32, kind="ExternalInput").ap()
    y_d = nc.dram_tensor("y", [T, D], F32, kind="ExternalOutput").ap()
    ident = make_ident(fw)
    lnw = fw.sb([128, D], F32, "lnw")
    fw.dma("sp", lnw[:], lnw_d.partition_broadcast(128), writes=["lnw"])
    if final:
        fnw = fw.sb([128, D], F32, "fnw")
        fw.dma("sp", fnw[:], fnw_d.partition_broadcast(128), writes=["lnw2"])
    cw = fw.sb([128, NFB, 3], F32, "cw")
    cbias = fw.sb([128, NFB], F32, "cbias")
    fw.dma("sp", cw[:], cw_d, writes=["cw"])
    fw.dma("sp", cbias[:], cb_d, writes=["cbias"])
    x1g = fw.sb([128, 4, D], F32, "x1g")
    hT = fw.sb([128, 16, 512], BF16, "hT")
    hTh = fw.sb([128, 16, 128], BF16, "hTh")
    actT = fw.sb([128, NFB, 512], BF16, "actT")
    gcarry = fw.sb([128, NFB, 2], F32, "gcarry")
    hbs = Rot([(fw.sb([128, D], BF16, "hb%d" % i), "hb%d" % i) for i in range(2)])
    scr = fw.sb([128, D], BF16, "scr")
    stat = fw.sb([128, 2], F32, "stat")
    tps = Rot([(fw.ps("tp%d" % i, (128, 512), BF16), "tp%d" % i) for i in range(2)])
    gps = Rot([(fw.ps("g%d" % i), "g%d" % i) for i in range(2)])
    ups = Rot([(fw.ps("u%d" % i), "u%d" % i) for i in range(2)])
    dps = Rot([(fw.ps("d%d" % i), "d%d" % i) for i in range(2)])
    evac = Rot(["act", "dve"])
    wgs = Rot([(fw.sb([128, 16, 128], BF16, "wg%d" % i), "wg%d" % i) for i in range(2)])
    wus = Rot([(fw.sb([128, 16, 128], BF16, "wu%d" % i), "wu%d" % i) for i in range(2)])
    wds = Rot([(fw.sb([128, NFB, 256], BF16, "wd%d" % i), "wd%d" % i) for i in range(2)])
    gsbs = Rot([(fw.sb([128, 514], F32, "gsb%d" % i), "gsb%d" % i) for i in range(2)])
    cvs = Rot([(fw.sb([128, 512], F32, "cv%d" % i), "cv%d" % i) for i in range(2)])
    sgs = Rot([(fw.sb([128, 512], F32, "sg%d" % i), "sg%d" % i) for i in range(2)])
    wgv = wg_d.rearrange("(k p) n -> p k n", p=128)
    wuv = wu_d.rearrange("(k p) n -> p k n", p=128)
    wdv = wd_d.rearrange("(f p) n -> p f n", p=128)
    fw.dma("sp", x1g[:, 0, :], xh_d, writes=[("x1g", 0)])
    hb, hkey = hbs.next()
    rmsnorm_tile(fw, x1g[:, 0, :], ("x1g", 0), lnw[:], hb[:], hkey, scr[:], "scr", stat, "stat")
    transpose_cols(fw, hb, hkey, 16, lambda j, n: hTh[:, j:j + n, :], ["hTh"], tps, ident, evac)
    for g in range(NT // 4):
        for tt in range(4):
            t = g * 4 + tt
            fw.dma("sp", x1g[:, tt, :], x_d[t * 128:(t + 1) * 128, :], writes=[("x1g", tt)])
            hb, hkey = hbs.next()
            rmsnorm_tile(fw, x1g[:, tt, :], ("x1g", tt), lnw[:], hb[:], hkey, scr[:], "scr", stat, "stat")
            transpose_cols(fw, hb, hkey, 16, lambda j, n, tt=tt: hT[:, j:j + n, tt * 128:(tt + 1) * 128],
                           [("hT", tt)], tps, ident, evac)
        hTall = [("hT", i) for i in range(4)]

        def load_w(fb):
            a, ak = wgs.next()
            b, bk = wus.next()
            fw.dma("pool", a[:], wgv[:, :, fb * 128:(fb + 1) * 128], writes=[ak])
            fw.dma("pool", b[:], wuv[:, :, fb * 128:(fb + 1) * 128], writes=[bk])
            return a, ak, b, bk

        nxt = load_w(0)
        for fb in range(NFB):
            wga, wgk, wua, wuk = nxt
            if fb + 1 < NFB:
                nxt = load_w(fb + 1)
            gp, gk = gps.next()
            up, uk = ups.next()
            gsb, gsk = gsbs.next()
            if g == 0:
                for k in range(16):
                    fw.op("pe", lambda e, k=k: e.matmul(gp[:, 0:2], lhsT=wga[:, k, :], rhs=hTh[:, k, 126:128],
                                                        start=(k == 0), stop=(k == 15)), reads=[wgk, "hTh"], writes=[gk])
                fw.op("dve", lambda e, fb=fb: e.tensor_copy(out=gcarry[:, fb, :], in_=gp[:, 0:2]), reads=[gk],
                      writes=[("gc", fb)])
            for k in range(16):
                fw.op("pe", lambda e, k=k: e.matmul(gp[:], lhsT=wga[:, k, :], rhs=hT[:, k, :], start=(k == 0),
                                                    stop=(k == 15)), reads=[wgk] + hTall, writes=[gk])
            for k in range(16):
                fw.op("pe", lambda e, k=k: e.matmul(up[:], lhsT=wua[:, k, :], rhs=hT[:, k, :], start=(k == 0),
                                                    stop=(k == 15)), reads=[wuk] + hTall, writes=[uk])
            fw.op("act", lambda e: e.activation(out=gsb[:, 2:514], in_=gp[:], func=AF.Copy), reads=[gk], writes=[gsk])
            fw.op("dve", lambda e, fb=fb: e.tensor_copy(out=gsb[:, 0:2], in_=gcarry[:, fb, :]), reads=[("gc", fb), gsk],
                  writes=[gsk])
            cv, cvk = cvs.next()
            fw.op("dve", lambda e, fb=fb: e.tensor_scalar(out=cv[:], in0=gsb[:, 2:514], scalar1=cw[:, fb, 2:3],
                                                          scalar2=cbias[:, fb:fb + 1], op0=ALU.mult, op1=ALU.add),
                  reads=[gsk, "cw", "cbias"], writes=[cvk])
            fw.op("dve", lambda e, fb=fb: e.scalar_tensor_tensor(out=cv[:], in0=gsb[:, 1:513], scalar=cw[:, fb, 1:2],
                                                                 in1=cv[:], op0=ALU.mult, op1=ALU.add),
                  reads=[gsk, "cw", cvk], writes=[cvk])
            fw.op("dve", lambda e, fb=fb: e.scalar_tensor_tensor(out=cv[:], in0=gsb[:, 0:512], scalar=cw[:, fb, 0:1],
                                                                 in1=cv[:], op0=ALU.mult, op1=ALU.add),
                  reads=[gsk, "cw", cvk], writes=[cvk])
            fw.op("dve", lambda e, fb=fb: e.tensor_copy(out=gcarry[:, fb, :], in_=gsb[:, 512:514]), reads=[gsk],
                  writes=[("gc", fb)])
            sg, sgk = sgs.next()
            fw.op("act", lambda e: e.activation(out=sg[:], in_=cv[:], func=AF.Silu), reads=[cvk], writes=[sgk])
            fw.op("dve", lambda e, fb=fb: e.tensor_tensor(out=actT[:, fb, :], in0=sg[:], in1=up[:], op=ALU.mult),
                  reads=[sgk, uk], writes=[("actT", fb)])
        actall = [("actT", fb) for fb in range(NFB)]

        def load_wd(c):
            a, ak = wds.next()
            fw.dma("pool", a[:], wdv[:, :, c * 256:(c + 1) * 256], writes=[ak])
            return a, ak

        nxt = load_wd(0)
        for c in range(8):
            wda, wdk = nxt
            if c + 1 < 8:
                nxt = load_wd(c + 1)
            for tt in range(4):
                dp, dk = dps.next()
                for fb in range(NFB):
                    fw.op("pe", lambda e, fb=fb, tt=tt: e.matmul(dp[:, 0:256], lhsT=actT[:, fb, tt * 128:(tt + 1) * 128],
                                                                rhs=wda[:, fb, :], start=(fb == 0), stop=(fb == NFB - 1)),
                          reads=[wdk] + (actall if fb == 0 else []), writes=[dk])
                fw.op("dve", lambda e, tt=tt, c=c, dp=dp: e.tensor_tensor(out=x1g[:, tt, c * 256:(c + 1) * 256],
                                                                          in0=x1g[:, tt, c * 256:(c + 1) * 256],
                                                                          in1=dp[:, 0:256], op=ALU.add),
                      reads=[("x1g", tt), dk], writes=[("x1g", tt)])
        for tt in range(4):
            t = g * 4 + tt
            if final:
                fw.op("act", lambda e, tt=tt: e.activation(out=scr[:], in_=x1g[:, tt, :], func=AF.Square,
                                                           accum_out=stat[:, 0:1]), reads=[("x1g", tt)],
                      writes=["scr", "stat"])
                fw.op("act", lambda e: e.activation(out=stat[:, 1:2], in_=stat[:, 0:1], func=AF.Sqrt, scale=1.0 / D,
                                                    bias=EPS), reads=["stat"], writes=["stat"])
                fw.op("dve", lambda e: e.reciprocal(out=stat[:, 1:2], in_=stat[:, 1:2]), reads=["stat"], writes=["stat"])
                fw.op("dve", lambda e, tt=tt: e.scalar_tensor_tensor(out=x1g[:, tt, :], in0=x1g[:, tt, :],
                                                                     scalar=stat[:, 1:2], in1=fnw[:], op0=ALU.mult,
                                                                     op1=ALU.mult),
                      reads=[("x1g", tt), "stat", "lnw2"], writes=[("x1g", tt)])
            fw.dma("sp", y_d[t * 128:(t + 1) * 128, :], x1g[:, tt, :], reads=[("x1g", tt)], lane="st_x1g%d" % tt)
    fw.finish()
    return nc


NCH = T // 64


def build_hgrn_a(dbg=99, ntiles=NT):
    nc = bass.Bass("TRN2", target_bir_lowering=False)
    fw = FW(nc)
    x_d = nc.dram_tensor("x", [T, D], F32, kind="ExternalInput").ap()
    lnw_d = nc.dram_tensor("lnw", [D], F32, kind="ExternalInput").ap()
    w_d = nc.dram_tensor("w_in", [D, HG_IN], F32, kind="ExternalInput").ap()
    lbl_d = nc.dram_tensor("lbl", [2, MW], F32, kind="ExternalInput").ap()
    mem_d = nc.dram_tensor("mem", [256, D], F32, kind="ExternalInput").ap()
    mnw_d = nc.dram_tensor("mnw", [D], F32, kind="ExternalInput").ap()
    wkv_d = nc.dram_tensor("wkv", [D, 1024], F32, kind="ExternalInput").ap()
    mtri_d = nc.dram_tensor("mtri", [128, 128], F32, kind="ExternalInput").ap()
    ustr_d = nc.dram_tensor("ustr", [128, 128], F32, kind="ExternalInput").ap()
    oloc_d = nc.dram_tensor("oloc", [T, MW], F32, kind="ExternalOutput").ap()
    qdT_d = nc.dram_tensor("qdT", [12, 128, T], BF16, kind="ExternalOutput").ap()
    gate_d = nc.dram_tensor("gate", [T, MW], BF16, kind="ExternalOutput").ap()
    sloc_d = nc.dram_tensor("sloc", [12, 128, 128], F32, kind="ExternalOutput").ap()
    dec_d = nc.dram_tensor("decT", [128, 12, NCH], F32, kind="ExternalOutput").ap()
    mo_d = nc.dram_tensor("memout", [T, 512], BF16, kind="ExternalOutput").ap()
    ptot_d = nc.dram_tensor("ptot", [128, 12], F32, kind="ExternalOutput").ap()

    ident = make_ident(fw)
    lnw = fw.sb([128, D], F32, "lnw")
    hT = fw.sb([128, 16, T], BF16, "hT")
    wbufs = Rot([(fw.sb([128, 16, 512], BF16, "wb%d" % i), "wb%d" % i) for i in range(3)])
    P = [(fw.ps("p%d" % i), "p%d" % i) for i in range(5)]
    tps = Rot([(fw.ps("tp0", (128, 512), BF16), "tp0")])
    ob = fw.ps("ob")
    mb = fw.ps("mb")
    evac = Rot(["act", "dve"])
    accs = Rot([P[0], P[1]])
    xt0 = fw.sb([128, D], F32, "xt0")
    hb0 = fw.sb([128, D], BF16, "hb0")
    scr = fw.sb([128, D], BF16, "scr")
    stat = fw.sb([128, 2], F32, "stat")
    memKT, memV = mem_kv_setup(fw, mem_d, mnw_d, wkv_d, ident, lnw, wbufs, accs, tps, evac, scratch=(xt0, scr, hb0))
    fw.dma("sp", lnw[:], lnw_d.partition_broadcast(128), writes=["lnw"])
    for t in range(NT):
        xt, xkey = xt0, "mem_x"
        hb, hkey = hb0, "mem_h"
        fw.dma("sp", xt[:], x_d[t * 128:(t + 1) * 128, :], writes=[xkey])
        rmsnorm_tile(fw, xt[:], xkey, lnw[:], hb[:], hkey, scr[:], "mem_scr", stat, "stat")
        transpose_cols(fw, hb, hkey, 16, lambda j, n, t=t: hT[:, j:j + n, t * 128:(t + 1) * 128],
                       [("hT", t)], tps, ident, evac)
    lbb = fw.sb([128, MW], F32, "lbb")
    oml = fw.sb([128, MW], F32, "oml")
    fw.dma("sp", lbb[:], lbl_d[1, :].partition_broadcast(128), writes=["lbb"])
    fw.dma("sp", oml[:], lbl_d[0, :].partition_broadcast(128), writes=["oml"])
    fw.op("dve", lambda e: e.tensor_tensor(out=lbb[:], in0=lbb[:], in1=oml[:], op=ALU.subtract), reads=["lbb", "oml"],
          writes=["lbb"])
    fw.op("act", lambda e: e.activation(out=lbb[:], in_=lbb[:], func=AF.Sigmoid), reads=["lbb"], writes=["lbb"])
    fw.op("dve", lambda e: e.tensor_scalar(out=oml[:], in0=lbb[:], scalar1=-1.0, scalar2=1.0, op0=ALU.mult, op1=ALU.add),
          reads=["lbb", "oml"], writes=["oml"])
    mtri = fw.sb([128, 128], F32, "mtri")
    ustr = fw.sb([128, 128], F32, "ustr")
    onesf = fw.sb([128, 1], F32, "onesf")
    fw.dma("sp", mtri[:], mtri_d, writes=["mtri"])
    fw.dma("sp", ustr[:], ustr_d, writes=["ustr"])
    fw.op("pool", lambda e: e.memset(onesf[:], 1.0), writes=["onesf"])
    S = fw.sb([128, 4, 128], F32, "S")
    Sb = fw.sb([128, 4, 128], BF16, "Sb")
    decs = fw.sb([128, 12, NCH], F32, "decs")
    ptot = fw.sb([128, 12], F32, "ptot")
    fw.op("dve", lambda e: e.memset(ptot[:], 1.0), writes=[("ptot", h) for h in range(12)])
    F = {n: fw.sb([128, 512], F32, "f_" + n) for n in ("sgf", "sq", "qs", "fg", "kin", "g", "eb", "enb", "erb")}
    B = {n: fw.sb([128, 512], BF16, "b_" + n) for n in ("qd", "kd", "vv", "gh", "gl")}
    mtrib = fw.sb([128, 128], BF16, "mtrib")
    ustrb = fw.sb([128, 128], BF16, "ustrb")
    onesb = fw.sb([128, 1], BF16, "onesb")
    fw.op("dve", lambda e: e.tensor_copy(out=mtrib[:], in_=mtri[:]), reads=["mtri"], writes=["mtrib"])
    fw.op("dve", lambda e: e.tensor_copy(out=ustrb[:], in_=ustr[:]), reads=["ustr"], writes=["ustrb"])
    fw.op("dve", lambda e: e.tensor_copy(out=onesb[:], in_=onesf[:]), reads=["onesf"], writes=["onesb"])
    cmf = fw.sb([128, 2], F32, "cmf")
    cmb = fw.sb([128, 2], BF16, "cmb")
    fw.op("dve", lambda e: e.tensor_copy(out=cmf[:, 0:1], in_=mtri[:, 63:64]), reads=["mtri"], writes=["cmf"])
    fw.op("dve", lambda e: e.tensor_copy(out=cmf[:, 1:2], in_=mtri[:, 127:128]), reads=["mtri", "cmf"], writes=["cmf"])
    fw.op("dve", lambda e: e.tensor_copy(out=cmb[:], in_=cmf[:]), reads=["cmf"], writes=["cmb"])
    khm = [fw.sb([128, 512], BF16, "khm%d" % i) for i in range(2)]
    qdTs = fw.sb([128, 4, 128], BF16, "qdTs")
    kdT = fw.sb([128, 4, 128], BF16, "kdT")
    Am = fw.sb([128, 128], BF16, "Am")
    ost = Rot([(fw.sb([128, 512], F32, "ost%d" % i), "ost%d" % i) for i in range(2)])
    wv = w_d.rearrange("(k p) n -> p k n", p=128)

    def elt(eng, out, okey, fn, reads):
        fw.op(eng, fn, reads=reads, writes=[okey])

    for hg in range(3 if dbg >= 1 else 0):
        ws = []
        for kind in range(3):
            wb, wkey = wbufs.next()
            c0 = kind * MW + hg * 512
            fw.dma("pool", wb[:], wv[:, :, c0:c0 + 512], writes=[wkey])
            ws.append((wb, wkey))
        fw.op("dve", lambda e: e.memset(S[:], 0.0), writes=[("S", i) for i in range(4)])
        fw.op("dve", lambda e: e.memset(Sb[:], 0.0), writes=[("Sb", i) for i in range(4)])
        cols = slice(hg * 512, (hg + 1) * 512)
        for t in range(ntiles):
            for kind in range(3):
                pp, pk = P[kind]
                wb, wkey = ws[kind]
                for k in range(16):
                    fw.op("pe", lambda e, k=k, pp=pp, wb=wb: e.matmul(pp[:], lhsT=hT[:, k, t * 128:(t + 1) * 128],
                                                                      rhs=wb[:, k, :], start=(k == 0), stop=(k == 15)),
                          reads=[wkey, ("hT", t)], writes=[pk])
            qp, fp, ip = P[0][0], P[1][0], P[2][0]
            elt("act", F["sgf"], "sgf", lambda e: e.activation(out=F["sgf"][:], in_=fp[:], func=AF.Sigmoid), ["p1"])
            elt("act", F["sq"], "sq", lambda e: e.activation(out=F["sq"][:], in_=qp[:], func=AF.Sigmoid), ["p0"])
            elt("act", B["vv"], "vv", lambda e: e.activation(out=B["vv"][:], in_=ip[:], func=AF.Copy), ["p2"])
            elt("dve", F["qs"], "qs", lambda e: e.tensor_tensor(out=F["qs"][:], in0=qp[:], in1=F["sq"][:], op=ALU.mult),
                ["p0", "sq"])
            elt("dve", F["fg"], "fg", lambda e: e.tensor_tensor(out=F["fg"][:], in0=F["sgf"][:], in1=oml[:, cols],
                                                                 op=ALU.mult), ["sgf", "oml"])
            elt("dve", F["fg"], "fg", lambda e: e.tensor_tensor(out=F["fg"][:], in0=F["fg"][:], in1=lbb[:, cols],
                                                                 op=ALU.add), ["fg", "lbb"])
            elt("dve", F["kin"], "kin", lambda e: e.tensor_scalar(out=F["kin"][:], in0=F["fg"][:], scalar1=-1.0,
                                                                   scalar2=1.0, op0=ALU.mult, op1=ALU.add), ["fg"])
            elt("act", F["g"], "g", lambda e: e.activation(out=F["g"][:], in_=F["fg"][:], func=AF.Ln), ["fg"])
            bc, rb = P[3][0], P[4][0]
            elt("dve", B["gh"], "gh", lambda e: e.tensor_copy(out=B["gh"][:], in_=F["g"][:]), ["g"])
            elt("dve", B["gl"], "gl", lambda e: e.tensor_tensor(out=B["gl"][:], in0=F["g"][:], in1=B["gh"][:],
                                                                 op=ALU.subtract), ["g", "gh"])
            fw.op("pe", lambda e: e.matmul(bc[:], lhsT=mtrib[:], rhs=B["gh"][:], start=True, stop=False),
                  reads=["gh", "mtrib"], writes=["p3"])
            fw.op("pe", lambda e: e.matmul(bc[:], lhsT=mtrib[:], rhs=B["gl"][:], start=False, stop=True),
                  reads=["gl", "mtrib"], writes=["p3"])
            fw.op("pe", lambda e: e.matmul(rb[:], lhsT=ustrb[:], rhs=B["gh"][:], start=True, stop=False),
                  reads=["gh", "ustrb"], writes=["p4"])
            fw.op("pe", lambda e: e.matmul(rb[:], lhsT=ustrb[:], rhs=B["gl"][:], start=False, stop=True),
                  reads=["gl", "ustrb"], writes=["p4"])
            elt("act", F["eb"], "eb", lambda e: e.activation(out=F["eb"][:], in_=bc[:], func=AF.Exp), ["p3"])
            elt("act", F["enb"], "enb", lambda e: e.activation(out=F["enb"][:], in_=bc[:], func=AF.Exp, scale=-1.0), ["p3"])
            elt("act", F["erb"], "erb", lambda e: e.activation(out=F["erb"][:], in_=rb[:], func=AF.Exp), ["p4"])
            elt("dve", B["qd"], "qd", lambda e: e.tensor_tensor(out=B["qd"][:], in0=F["qs"][:], in1=F["eb"][:], op=ALU.mult),
                ["qs", "eb"])
            elt("dve", B["kd"], "kd", lambda e: e.tensor_tensor(out=B["kd"][:], in0=F["kin"][:], in1=F["enb"][:],
                                                                 op=ALU.mult), ["kin", "enb"])
            for c2 in range(2):
                elt("dve", khm[c2], "khm%d" % c2, lambda e, c2=c2: e.scalar_tensor_tensor(
                    out=khm[c2][:], in0=F["kin"][:], scalar=cmf[:, c2:c2 + 1], in1=F["erb"][:], op0=ALU.mult,
                    op1=ALU.mult), ["kin", "erb", "cmf"])
            if dbg < 2:
                continue
            transpose_cols(fw, B["qd"], "qd", 4, lambda j, n: qdTs[:, j:j + n, :], ["qdTs"], tps, ident, evac)
            transpose_cols(fw, B["kd"], "kd", 4, lambda j, n: kdT[:, j:j + n, :], ["kdT"], tps, ident, evac)
            fw.dma("sp", qdT_d[hg * 4:(hg + 1) * 4, :, t * 128:(t + 1) * 128].rearrange("h d t -> d h t"), qdTs[:],
                   reads=["qdTs"], lane="st_qdTs")
            os_, oskey = ost.next()
            for hh in range(4 if dbg >= 3 else 0):
                hc = slice(hh * 128, (hh + 1) * 128)
                h = hg * 4 + hh
                fw.op("pe", lambda e, hh=hh: e.matmul(mb[:, 0:128], lhsT=kdT[:, hh, :], rhs=qdTs[:, hh, :], start=True,
                                                      stop=True), reads=["kdT", "qdTs"], writes=["mbA"])
                fw.op("dve", lambda e: e.tensor_tensor(out=Am[:], in0=mb[:, 0:128], in1=mtri[:], op=ALU.mult),
                      reads=["mbA", "mtri"], writes=["Am"])
                if dbg < 3.2:
                    continue
                fw.op("pe", lambda e, hc=hc: e.matmul(ob[:, 0:128], lhsT=Am[:], rhs=B["vv"][:, hc], start=True, stop=False),
                      reads=["Am", "vv"], writes=["ob"])
                if dbg < 3.3:
                    continue
                for c2 in range(2):
                    rows = slice(c2 * 64, (c2 + 1) * 64)
                    ci = 2 * t + c2
                    fw.op("pe", lambda e, hh=hh, rows=rows: e.matmul(ob[rows, 0:128], lhsT=qdTs[:, hh, rows],
                                                                    rhs=Sb[:, hh, :], start=False, stop=(rows.start == 64)),
                          reads=["qdTs", ("Sb", hh)], writes=["ob"])
                    if dbg < 3.4:
                        continue
                    fw.op("pe", lambda e, hc=hc, c2=c2: e.matmul(mb[:, 384:385], lhsT=B["gh"][:, hc],
                                                                rhs=cmb[:, c2:c2 + 1], start=True, stop=False),
                          reads=["gh", "cmb"], writes=["mbD"])
                    fw.op("pe", lambda e, hc=hc, c2=c2: e.matmul(mb[:, 384:385], lhsT=B["gl"][:, hc],
                                                                rhs=cmb[:, c2:c2 + 1], start=False, stop=True),
                          reads=["gl", "cmb"], writes=["mbD"])
                    fw.op("act", lambda e, h=h, ci=ci: e.activation(out=decs[:, h, ci:ci + 1], in_=mb[:, 384:385],
                                                                   func=AF.Exp), reads=["mbD"], writes=[("dec", h)])
                    if dbg < 3.5:
                        continue
                    fw.op("dve", lambda e, h=h, ci=ci: e.tensor_tensor(out=ptot[:, h:h + 1], in0=ptot[:, h:h + 1],
                                                                      in1=decs[:, h, ci:ci + 1], op=ALU.mult),
                          reads=[("ptot", h), ("dec", h)], writes=[("ptot", h)])
                    if dbg < 3.6:
                        continue
                    fw.op("pe", lambda e, hc=hc, c2=c2: e.matmul(mb[:, 256:384], lhsT=khm[c2][:, hc],
                                                                rhs=B["vv"][:, hc], start=True, stop=True),
                          reads=["khm%d" % c2, "vv"], writes=["mbS"])
                    if dbg < 3.7:
                        continue
                    fw.op("dve", lambda e, hh=hh, h=h, ci=ci: e.scalar_tensor_tensor(
                        out=S[:, hh, :], in0=S[:, hh, :], scalar=decs[:, h, ci:ci + 1], in1=mb[:, 256:384], op0=ALU.mult,
                        op1=ALU.add), reads=[("S", hh), ("dec", h), "mbS"], writes=[("S", hh)])
                    if dbg < 3.8:
                        continue
                    fw.op("dve", lambda e, hh=hh: e.tensor_copy(out=Sb[:, hh, :], in_=S[:, hh, :]),
                          reads=[("S", hh)], writes=[("Sb", hh)])
                fw.op("dve", lambda e, hc=hc, os_=os_: e.tensor_copy(out=os_[:, hc], in_=ob[:, 0:128]),
                      reads=["ob"], writes=[oskey])
            fw.dma("sp", oloc_d[t * 128:(t + 1) * 128, cols], os_[:], reads=[oskey], lane="st_" + oskey)
        for hh in range(4):
            fw.dma("sp", sloc_d[hg * 4 + hh, :, :], S[:, hh, :], reads=[("S", hh)], lane="st_S")
    fw.dma("sp", dec_d, decs[:], reads=[("dec", h) for h in range(12)], lane="st_dec")
    fw.dma("sp", ptot_d, ptot[:], reads=[("ptot", h) for h in range(12)], lane="st_ptot")
    gst = Rot([(fw.sb([128, 512], BF16, "gst%d" % i), "gst%d" % i) for i in range(2)])
    mqT = fw.sb([128, 4, 128], BF16, "mqT")
    E = fw.sb([128, 8, 128], BF16, "memE")
    rden = fw.sb([128, 4], F32, "rden")
    for bi in range(4 if dbg >= 4 else 0):
        wb, wkey = wbufs.next()
        c0 = 3 * MW + bi * 512
        fw.dma("pool", wb[:], wv[:, :, c0:c0 + 512], writes=[wkey])
        for t in range(NT):
            pp, pk = P[t % 2]
            for k in range(16):
                fw.op("pe", lambda e, k=k, pp=pp: e.matmul(pp[:], lhsT=hT[:, k, t * 128:(t + 1) * 128], rhs=wb[:, k, :],
                                                           start=(k == 0), stop=(k == 15)),
                      reads=[wkey, ("hT", t)], writes=[pk])
            gs, gkey = gst.next()
            if bi < 3:
                fw.op("act", lambda e, pp=pp, gs=gs: e.activation(out=gs[:], in_=pp[:], func=AF.Silu), reads=[pk],
                      writes=[gkey])
                fw.dma("sp", gate_d[t * 128:(t + 1) * 128, bi * 512:(bi + 1) * 512], gs[:], reads=[gkey], lane="st_" + gkey)
            else:
                fw.op("act", lambda e, pp=pp, gs=gs: e.activation(out=gs[:], in_=pp[:], func=AF.Copy), reads=[pk],
                      writes=[gkey])
                transpose_cols(fw, gs, gkey, 4, lambda j, n: mqT[:, j:j + n, :], ["mqT"], tps, ident, evac)
                mo, mokey = gst.next()
                mem_attention(fw, mqT, "mqT", memKT, memV, P[2][0], P[3][0], P[4][0], ob, E, "memE", rden, "rden", mo,
                              mokey, pk=("p2", "p3", "p4", "ob"))
                fw.dma("sp", mo_d[t * 128:(t + 1) * 128, :], mo[:], reads=[mokey], lane="st_" + mokey)
    fw.finish()
    nc._fw = fw
    return nc


def build_hgrn_b():
    nc = bass.Bass("TRN2", target_bir_lowering=False)
    fw = FW(nc)
    oloc_d = nc.dram_tensor("oloc", [T, MW], F32, kind="ExternalInput").ap()
    qdT_d = nc.dram_tensor("qdT", [12, 128, T], BF16, kind="ExternalInput").ap()
    gate_d = nc.dram_tensor("gate", [T, MW], BF16, kind="ExternalInput").ap()
    dec_d = nc.dram_tensor("decT", [128, 12, NCH], F32, kind="ExternalInput").ap()
    sp_d = nc.dram_tensor("sprev", [7, 12, 128, 128], F32, kind="ExternalInput").ap()
    pp_d = nc.dram_tensor("pprev", [7, 128, 12], F32, kind="ExternalInput").ap()
    gnw_d = nc.dram_tensor("gnw", [128], F32, kind="ExternalInput").ap()
    mix_d = nc.dram_tensor("mix", [T, MW], BF16, kind="ExternalOutput").ap()
    S = fw.sb([128, 12, 128], F32, "S")
    Sb = fw.sb([128, 12, 128], BF16, "Sb")
    spt = Rot([(fw.sb([128, 12, 128], F32, "spt%d" % i), "spt%d" % i) for i in range(2)])
    ppt = Rot([(fw.sb([128, 12], F32, "ppt%d" % i), "ppt%d" % i) for i in range(2)])
    decs = fw.sb([128, 12, NCH], F32, "decs")
    gnw = fw.sb([128, 128], F32, "gnw")
    fw.dma("sp", decs[:], dec_d, writes=["decs"])
    fw.dma("sp", gnw[:], gnw_d.partition_broadcast(128), writes=["gnw"])
    fw.op("dve", lambda e: e.memset(S[:], 0.0), writes=["S"])
    for j in range(7):
        sp, spk = spt.next()
        pp, ppk = ppt.next()
        fw.dma("sp", sp[:], sp_d[j].rearrange("h k v -> k h v"), writes=[spk])
        fw.dma("sp", pp[:], pp_d[j], writes=[ppk])
        fw.op("dve", lambda e, pp=pp: e.tensor_tensor(out=S[:], in0=S[:], in1=pp[:].unsqueeze(2).to_broadcast([128, 12, 128]),
                                                      op=ALU.mult), reads=["S", ppk], writes=["S"])
        fw.op("dve", lambda e, sp=sp: e.tensor_tensor(out=S[:], in0=S[:], in1=sp[:], op=ALU.add), reads=["S", spk],
              writes=["S"])
    C = [(fw.ps("c%d" % i), "c%d" % i) for i in range(3)]
    qts = Rot([(fw.sb([128, 12, 128], BF16, "qt%d" % i), "qt%d" % i) for i in range(2)])
    ols = Rot([(fw.sb([128, MW], F32, "ol%d" % i), "ol%d" % i) for i in range(2)])
    gts = Rot([(fw.sb([128, MW], BF16, "gt%d" % i), "gt%d" % i) for i in range(2)])
    mxs = Rot([(fw.sb([128, MW], BF16, "mx%d" % i), "mx%d" % i) for i in range(2)])
    sq = fw.sb([128, 128], F32, "sqj")
    ssq = fw.sb([128, 12], F32, "ssq")
    for t in range(NT):
        tok = slice(t * 128, (t + 1) * 128)
        qt, qk = qts.next()
        ol, olk = ols.next()
        gt, gk = gts.next()
        fw.dma("sp", qt[:], qdT_d[:, :, tok].rearrange("h d t -> d h t"), writes=[qk])
        fw.dma("sp", ol[:], oloc_d[tok, :], writes=[olk])
        fw.dma("sp", gt[:], gate_d[tok, :], writes=[gk])
        for c2 in range(2):
            rows = slice(c2 * 64, (c2 + 1) * 64)
            ci = 2 * t + c2
            fw.op("act", lambda e: e.activation(out=Sb[:], in_=S[:], func=AF.Copy), reads=["S"], writes=["Sb"])
            for h in range(12):
                cp, ck = C[h // 4]
                fw.op("pe", lambda e, h=h, rows=rows, cp=cp: e.matmul(cp[rows, (h % 4) * 128:(h % 4 + 1) * 128],
                                                                     lhsT=qt[:, h, rows], rhs=Sb[:, h, :], start=True,
                                                                     stop=True), reads=[qk, "Sb"], writes=[ck])
            fw.op("dve", lambda e, ci=ci: e.tensor_tensor(out=S[:], in0=S[:],
                                                          in1=decs[:, :, ci:ci + 1].to_broadcast([128, 12, 128]),
                                                          op=ALU.mult), reads=["S", "decs"], writes=["S"])
        for g in range(3):
            cp, ck = C[g]
            fw.op("dve", lambda e, g=g, cp=cp: e.tensor_tensor(out=ol[:, g * 512:(g + 1) * 512],
                                                               in0=ol[:, g * 512:(g + 1) * 512], in1=cp[:], op=ALU.add),
                  reads=[olk, ck], writes=[olk])
        for h in range(12):
            fw.op("act", lambda e, h=h: e.activation(out=sq[:], in_=ol[:, h * 128:(h + 1) * 128], func=AF.Square,
                                                     accum_out=ssq[:, h:h + 1]), reads=[olk], writes=["sqj", ("ssq", h)])
        allssq = [("ssq", h) for h in range(12)]
        fw.op("act", lambda e: e.activation(out=ssq[:], in_=ssq[:], func=AF.Sqrt, scale=1.0 / 128, bias=EPS),
              reads=allssq, writes=["ssq2"])
        fw.op("dve", lambda e: e.reciprocal(out=ssq[:], in_=ssq[:]), reads=["ssq2"], writes=["ssq3"])
        olv = ol[:, :].rearrange("p (h d) -> p h d", d=128)
        fw.op("dve", lambda e: e.tensor_tensor(out=olv, in0=olv, in1=ssq[:].unsqueeze(2).to_broadcast([128, 12, 128]),
                                               op=ALU.mult), reads=[olk, "ssq3"], writes=[olk] + allssq)
        fw.op("dve", lambda e: e.tensor_tensor(out=olv, in0=olv, in1=gnw[:].unsqueeze(1).to_broadcast([128, 12, 128]),
                                               op=ALU.mult), reads=[olk, "gnw"], writes=[olk])
        mx, mxk = mxs.next()
        fw.op("dve", lambda e: e.tensor_tensor(out=mx[:], in0=ol[:], in1=gt[:], op=ALU.mult), reads=[olk, gk],
              writes=[mxk])
        fw.dma("sp", mix_d[tok, :], mx[:], reads=[mxk], lane="st_" + mxk)
    fw.finish()
    return nc


_PROGS = {}


def _run(name, builder, in_maps):
    if name not in _PROGS:
        _PROGS[name] = builder()
    res = run_bass_kernel_spmd(_PROGS[name], in_maps, core_ids=list(range(NCORES)))
    return [{k: np.asarray(v) for k, v in r.items()} for r in res.results]


def _c(a):
    return np.ascontiguousarray(a)


def kernel(x, mem, positions, ln1_w, ln2_w, dsa_w_in, hgrn_w_in, hgrn_lb_logits, hgrn_gnorm_w, mem_norm_w, w_mem_kv,
           w_out, ffn_w_gate, ffn_w_up, ffn_conv_w, ffn_conv_b, ffn_w_down, final_norm_w):
    f32 = np.float32
    x0 = np.asarray(x, f32)[0]
    memf = _c(np.asarray(mem, f32)[0])
    pos = np.asarray(positions)[0].astype(np.int32)
    invf = rope_invf()
    mnw = _c(np.asarray(mem_norm_w, f32))
    wkv = _c(np.asarray(w_mem_kv, f32))
    r1 = _run("proj0", build_proj0, [
        {"x": _c(x0[c * T:(c + 1) * T]), "pos": _c(pos[c * T:(c + 1) * T].reshape(NT, 128).T), "invf": invf,
         "lnw": _c(np.asarray(ln1_w, f32)[0]), "w_in": _c(np.asarray(dsa_w_in, f32)[0]), "mem": memf, "mnw": mnw,
         "wkv": wkv} for c in range(NCORES)])
    kT_all = _c(np.concatenate([r["kT"] for r in r1], axis=2))
    qT_all = np.concatenate([r["qT"] for r in r1], axis=2)
    iqT_all = np.concatenate([r["iqT"] for r in r1], axis=2)
    ikT_all = _c(np.concatenate([r["ikT"] for r in r1], axis=1))
    v_all = _c(np.concatenate([r["v"] for r in r1], axis=0))
    iw_all = np.concatenate([r["iw"] for r in r1], axis=0)
    memout0 = np.concatenate([r["memout"] for r in r1], axis=0)
    ctab = np.zeros((128, 32), f32)
    ctab[:, :] = (2.0 ** -(np.arange(32) + 1)).astype(f32)
    in2 = []
    toks_of = []
    for c in range(NCORES):
        toks = np.concatenate([np.arange((8 * j + c) * 128, (8 * j + c + 1) * 128) for j in range(NT)])
        toks_of.append(toks)
        cm = np.full((128, 1024), -1e30, f32)
        for r in range(8):
            if r < c:
                cm[:, r * 128:(r + 1) * 128] = 0
            elif r == c:
                cm[:, r * 128:(r + 1) * 128] = np.where(np.arange(128)[None, :] <= np.arange(128)[:, None], 0, -1e30)
        in2.append({"qT": _c(qT_all[:, :, toks]), "iqT": _c(iqT_all[:, :, toks]), "iw": _c(iw_all[toks]),
                    "kT": kT_all, "v": v_all, "ikT": ikT_all, "cmask": cm, "ctab": ctab})
    r2 = _run("attn0", build_attn0, in2)
    cat = np.zeros((SEQ, D), NPBF)
    for c in range(NCORES):
        cat[toks_of[c], :MW] = r2[c]["mix"]
    cat[:, MW:] = memout0
    xcur = x0
    for layer in range(2):
        if layer == 1:
            mtri = np.zeros((128, 128), f32)
            ustr = np.zeros((128, 128), f32)
            for a in range(128):
                for b in range(128):
                    if a // 64 == b // 64:
                        if a <= b:
                            mtri[a, b] = 1
                        if a > b:
                            ustr[a, b] = 1
            ra = _run("hgrn_a", build_hgrn_a, [
                {"x": _c(xcur[c * T:(c + 1) * T]), "lnw": _c(np.asarray(ln1_w, f32)[1]),
                 "w_in": _c(np.asarray(hgrn_w_in, f32)[0]), "lbl": _c(np.asarray(hgrn_lb_logits, f32)), "mem": memf,
                 "mnw": mnw, "wkv": wkv, "mtri": mtri, "ustr": ustr} for c in range(NCORES)])
            inb = []
            for c in range(NCORES):
                sp = np.zeros((7, 12, 128, 128), f32)
                pp = np.zeros((7, 128, 12), f32)
                for j in range(7):
                    src = c - 7 + j
                    if src >= 0:
                        sp[j] = ra[src]["sloc"]
                        pp[j] = ra[src]["ptot"]
                inb.append({"oloc": ra[c]["oloc"], "qdT": ra[c]["qdT"], "gate": ra[c]["gate"], "decT": ra[c]["decT"],
                            "sprev": sp, "pprev": pp, "gnw": _c(np.asarray(hgrn_gnorm_w, f32)[0])})
            rb = _run("hgrn_b", build_hgrn_b, inb)
            cat = np.zeros((SEQ, D), NPBF)
            for c in range(NCORES):
                cat[c * T:(c + 1) * T, :MW] = rb[c]["mix"]
                cat[c * T:(c + 1) * T, MW:] = ra[c]["memout"]
        r3 = _run("outproj", build_outproj, [
            {"x": _c(xcur[c * T:(c + 1) * T]), "cat": _c(cat[c * T:(c + 1) * T]),
             "w_out": _c(np.asarray(w_out, f32)[layer])} for c in range(NCORES)])
        x1 = np.concatenate([r["x1"] for r in r3], axis=0)
        cwl = np.asarray(ffn_conv_w, f32)[layer]
        cw_l = _c(cwl.reshape(3, NFB, 128).transpose(2, 1, 0))
        cb_l = _c(np.asarray(ffn_conv_b, f32)[layer].reshape(NFB, 128).T)
        in4 = []
        for c in range(NCORES):
            xh = np.zeros((128, D), f32)
            if c > 0:
                xh[126:128] = x1[c * T - 2:c * T]
            in4.append({"x1": _c(x1[c * T:(c + 1) * T]), "x1h": xh, "lnw": _c(np.asarray(ln2_w, f32)[layer]),
                        "fnw": _c(np.asarray(final_norm_w, f32)), "wg": _c(np.asarray(ffn_w_gate, f32)[layer]),
                        "wu": _c(np.asarray(ffn_w_up, f32)[layer]), "wd": _c(np.asarray(ffn_w_down, f32)[layer]),
                        "cw": cw_l, "cb": cb_l})
        final = (layer == 1)
        r4 = _run("ffn%d" % int(final), (lambda: build_ffn(True)) if final else (lambda: build_ffn(False)), in4)
        xcur = np.concatenate([r["y"] for r in r4], axis=0)
    return xcur[None].astype(f32)
```
